# Optimizing a Trainium2 kernel written in Bass

```python
import jax
import jax.numpy as jnp
from jax import lax
import numpy as np

D_MODEL = 1024
BATCH = 8
SEQ = 4096
DEPTH = 4

GRID_W = 64
CTX_LEN = 256
DN_HEADS = 8
DN_DK = 64
DN_W = DN_HEADS * DN_DK
DN_CHUNK = 64
CONV_K = 5
A_MIN = 1.0
A_MAX = 16.0
DT_MIN = 1e-3
DT_MAX = 1e-1
NA_HEADS = 8
NA_DH = 64
NA_W = NA_HEADS * NA_DH
WIN_R = 8
WIN_C = 16
QB_W = 16
CB_W = QB_W + WIN_C
ROPE_BASE = 10000.0
PEER_HEADS = 8
N_KEYS = 128
N_EXPERTS = N_KEYS * N_KEYS
PEER_DK = 128
PEER_TOPK = 16
PEER_BLOCK = 128
IN_W = 3 * DN_W + DN_W + 4 * DN_HEADS + 3 * NA_W + 2 * D_MODEL
N_MOD = 6
EPS = 1e-6
NEG_INF = -1e30

kernel_name = 'hybrid_deltanet_natten_peer_dit'


def _rmsnorm(x, w):
    xf = x.astype(jnp.float32)
    y = xf * lax.rsqrt(jnp.mean(xf * xf, axis=-1, keepdims=True) + EPS)
    return (y * w.astype(jnp.float32)).astype(x.dtype)


def _l2norm(x):
    xf = x.astype(jnp.float32)
    return xf * lax.rsqrt(jnp.sum(xf * xf, axis=-1, keepdims=True) + EPS)


def _modulate(h, shift, scale):
    return h * (1 + scale) + shift


def _axial_rotary(x):
    L, hd = x.shape[1], x.shape[-1]
    n_freq = hd // 4
    t = jnp.arange(L)
    row = (t // GRID_W).astype(jnp.float32)
    col = (t % GRID_W).astype(jnp.float32)
    inv_freq = ROPE_BASE ** (-jnp.arange(n_freq, dtype=jnp.float32) / n_freq)
    ang = jnp.concatenate([row[:, None] * inv_freq, col[:, None] * inv_freq], axis=-1)
    cos = jnp.cos(ang)[None, :, None, :].astype(x.dtype)
    sin = jnp.sin(ang)[None, :, None, :].astype(x.dtype)
    x1, x2 = jnp.split(x, 2, axis=-1)
    return jnp.concatenate([x1 * cos - x2 * sin, x2 * cos + x1 * sin], axis=-1)


def _short_conv(x, w):
    ch = x.shape[-1]
    y = lax.conv_general_dilated(
        x, w[:, None, :].astype(x.dtype), window_strides=(1,),
        padding=[(CONV_K // 2, CONV_K // 2)],
        dimension_numbers=('NWC', 'WIO', 'NWC'), feature_group_count=ch)
    return jax.nn.silu(y)


def _flip_if(t, rev):
    return jnp.flip(t, axis=2) if rev else t


def _gated_delta_chunked(q, k, v, g, beta, s0):
    B, H, L, dk = q.shape
    dv = v.shape[-1]
    n = L // DN_CHUNK

    def to_chunks(t):
        return t.reshape(B, H, n, DN_CHUNK, *t.shape[3:])

    q = to_chunks(q * dk ** -0.5)
    k = to_chunks(k)
    v = to_chunks(v)
    beta = to_chunks(beta)
    g = jnp.cumsum(to_chunks(g), axis=-1)
    incl = jnp.tril(jnp.ones((DN_CHUNK, DN_CHUNK), dtype=bool))
    strict = jnp.tril(jnp.ones((DN_CHUNK, DN_CHUNK), dtype=bool), k=-1)
    diff = g[..., :, None] - g[..., None, :]
    decay = jnp.where(incl, jnp.exp(jnp.where(incl, diff, 0.0)), 0.0)
    k_beta = k * beta[..., None]
    a_mat = jnp.where(strict, jnp.einsum('bhncd,bhnsd->bhncs', k_beta, k) * decay, 0.0)
    eye = jnp.eye(DN_CHUNK, dtype=jnp.float32)
    t_mat = lax.linalg.triangular_solve(
        eye + a_mat, jnp.broadcast_to(eye, a_mat.shape),
        left_side=True, lower=True, unit_diagonal=True)
    w_val = jnp.einsum('bhncs,bhnse->bhnce', t_mat, v * beta[..., None])
    u_key = jnp.einsum('bhncs,bhnsd->bhncd', t_mat, k_beta * jnp.exp(g)[..., None])
    intra = jnp.where(incl, jnp.einsum('bhncd,bhnsd->bhncs', q, k) * decay, 0.0)
    g_last = g[..., -1]
    q_dec = q * jnp.exp(g)[..., None]
    k_dec = k * jnp.exp(g_last[..., None] - g)[..., None]
    xs = tuple(jnp.moveaxis(t, 2, 0) for t in (q_dec, k_dec, w_val, u_key, intra, g_last))

    def step(s, inp):
        qd, kd, wv, uk, att, gl = inp
        v_new = wv - jnp.einsum('bhcd,bhde->bhce', uk, s)
        o = jnp.einsum('bhcd,bhde->bhce', qd, s) + jnp.einsum('bhcs,bhse->bhce', att, v_new)
        s = s * jnp.exp(gl)[..., None, None] + jnp.einsum('bhcd,bhce->bhde', kd, v_new)
        return s, o

    s_final, o = lax.scan(step, s0, xs)
    o = jnp.moveaxis(o, 0, 2).reshape(B, H, L, dv)
    return o, s_final


def _bidir_delta(qc, kc, vc, gc, bc, qx, kx, vx, gx, bx):
    B, H, _, dk = qc.shape
    s0 = jnp.zeros((B, H, dk, vc.shape[-1]), jnp.float32)
    out_c, out_x = [], []
    for d in range(2):
        rev = d == 1
        oc, sc = _gated_delta_chunked(_flip_if(qc, rev), _flip_if(kc, rev), _flip_if(vc, rev),
                                      _flip_if(gc[d], rev), _flip_if(bc[d], rev), s0)
        ox, _ = _gated_delta_chunked(_flip_if(qx, rev), _flip_if(kx, rev), _flip_if(vx, rev),
                                     _flip_if(gx[d], rev), _flip_if(bx[d], rev), sc)
        out_c.append(_flip_if(oc, rev))
        out_x.append(_flip_if(ox, rev))
    return out_c[0] + out_c[1], out_x[0] + out_x[1]


def _delta_inputs(qkv, a, b, conv_w, a_log, dt_bias, rotary):
    B, L, _ = qkv.shape
    qkv = _short_conv(qkv, conv_w)
    q, k, v = (t.reshape(B, L, DN_HEADS, DN_DK) for t in jnp.split(qkv, 3, axis=-1))
    q, k = _l2norm(q), _l2norm(k)
    if rotary:
        q, k = _axial_rotary(q), _axial_rotary(k)

    def per_dir(t):
        return t.astype(jnp.float32).reshape(B, L, 2, DN_HEADS).transpose(2, 0, 3, 1)

    g = -jnp.exp(a_log.astype(jnp.float32))[:, None, :, None] * jax.nn.softplus(
        per_dir(a) + dt_bias.astype(jnp.float32)[:, None, :, None])
    beta = jax.nn.sigmoid(per_dir(b))

    def to_bhl(t):
        return t.astype(jnp.float32).transpose(0, 2, 1, 3)

    return to_bhl(q), to_bhl(k), to_bhl(v), g, beta


def _gated_head_norm(o, z, w):
    B, H, L, dv = o.shape
    o = _rmsnorm(o.transpose(0, 2, 1, 3), w)
    gate = jax.nn.silu(z.reshape(B, L, H, dv).astype(jnp.float32))
    return (o * gate).reshape(B, L, H * dv).astype(z.dtype)


def _na_inputs(qkv, q_w, k_w):
    B, L, _ = qkv.shape
    q, k, v = (t.reshape(B, L, NA_HEADS, NA_DH) for t in jnp.split(qkv, 3, axis=-1))
    q, k = _rmsnorm(q, q_w), _rmsnorm(k, k_w)
    return tuple(t.transpose(0, 2, 1, 3) for t in (q, k, v))


def _column_bands():
    ncb = GRID_W // QB_W
    q_col = np.arange(ncb)[:, None] * QB_W + np.arange(QB_W)[None, :]
    band_start = np.clip(np.arange(ncb) * QB_W - WIN_C // 2, 0, GRID_W - CB_W)
    key_col = band_start[:, None] + np.arange(CB_W)[None, :]
    win_start = np.clip(q_col - WIN_C // 2, 0, GRID_W - WIN_C)
    rel = key_col[:, None, :] - win_start[:, :, None]
    mask = (rel >= 0) & (rel < WIN_C)
    off = np.clip(key_col[:, None, :] - q_col[:, :, None] + (WIN_C - 1), 0, 2 * WIN_C - 2)
    return key_col, mask, off


def _neighbourhood_attention(q, k, v, k_ctx, v_ctx, rpb):
    B, H, L, hd = q.shape
    rows = L // GRID_W
    wr = min(WIN_R, rows)
    ncb = GRID_W // QB_W
    n_loc = wr * CB_W
    key_col, col_mask, col_off = _column_bands()
    mask = np.broadcast_to(col_mask[:, :, None, :], (ncb, QB_W, wr, CB_W)).reshape(ncb, QB_W, n_loc)
    qg = q.reshape(B, H, rows, GRID_W, hd)
    kg = k.reshape(B, H, rows, GRID_W, hd)
    vg = v.reshape(B, H, rows, GRID_W, hd)
    rpb_c = rpb[:, :, col_off]
    scale = hd ** -0.5

    def gather_band(t, rs):
        band = lax.dynamic_slice_in_dim(t, rs, wr, axis=2)[:, :, :, key_col]
        return band.transpose(0, 1, 3, 2, 4, 5).reshape(B, H, ncb, n_loc, hd)

    def row_block(r):
        rs = jnp.clip(r - wr // 2, 0, rows - wr)
        qb = lax.dynamic_index_in_dim(qg, r, axis=2, keepdims=False).reshape(B, H, ncb, QB_W, hd)
        kb, vb = gather_band(kg, rs), gather_band(vg, rs)
        dr = rs - r + jnp.arange(wr) + (WIN_R - 1)
        bias = rpb_c[:, dr].transpose(0, 2, 3, 1, 4).reshape(H, ncb, QB_W, n_loc)
        s_loc = jnp.einsum('bhjqd,bhjkd->bhjqk', qb, kb).astype(jnp.float32) * scale + bias
        s_loc = jnp.where(mask, s_loc, NEG_INF)
        s_ctx = jnp.einsum('bhjqd,bhkd->bhjqk', qb, k_ctx).astype(jnp.float32) * scale
        p = jax.nn.softmax(jnp.concatenate([s_loc, s_ctx], axis=-1), axis=-1).astype(v.dtype)
        o = (jnp.einsum('bhjqk,bhjkd->bhjqd', p[..., :n_loc], vb)
             + jnp.einsum('bhjqk,bhkd->bhjqd', p[..., n_loc:], v_ctx))
        return o.reshape(B, H, GRID_W, hd)

    o = lax.map(row_block, jnp.arange(rows))
    return o.transpose(1, 0, 3, 2, 4).reshape(B, L, H * hd)


def _context_attention(q, k, v):
    B, H, n, hd = q.shape
    s = jnp.einsum('bhqd,bhkd->bhqk', q, k).astype(jnp.float32) * hd ** -0.5
    p = jax.nn.softmax(s, axis=-1).astype(v.dtype)
    o = jnp.einsum('bhqk,bhkd->bhqd', p, v)
    return o.transpose(0, 2, 1, 3).reshape(B, n, H * hd)


def _merge(o_a, o_b, gate_a, gate_b, w_pa, w_pb, w_out):
    y = jax.nn.sigmoid(gate_a) * (o_a @ w_pa) + jax.nn.sigmoid(gate_b) * (o_b @ w_pb)
    return y @ w_out


def _split_in(p):
    sizes = [3 * DN_W, DN_W, 2 * DN_HEADS, 2 * DN_HEADS, 3 * NA_W, D_MODEL]
    return jnp.split(p, np.cumsum(sizes), axis=-1)


def _hybrid_mixer(hx, hc, w_in, conv_w, a_log, dt_bias, dn_norm_w, q_w, k_w, rpb,
                  w_pa, w_pb, w_out, with_ctx):
    dqkv_x, z_x, a_x, b_x, nqkv_x, ga_x, gb_x = _split_in(hx @ w_in)
    dqkv_c, z_c, a_c, b_c, nqkv_c, ga_c, gb_c = _split_in(hc @ w_in)
    dn_x = _delta_inputs(dqkv_x, a_x, b_x, conv_w, a_log, dt_bias, True)
    dn_c = _delta_inputs(dqkv_c, a_c, b_c, conv_w, a_log, dt_bias, False)
    o_dn_c, o_dn_x = _bidir_delta(*dn_c, *dn_x)
    qx, kx, vx = _na_inputs(nqkv_x, q_w, k_w)
    qc, kc, vc = _na_inputs(nqkv_c, q_w, k_w)
    o_na_x = _neighbourhood_attention(qx, kx, vx, kc, vc, rpb)
    y_x = _merge(_gated_head_norm(o_dn_x, z_x, dn_norm_w), o_na_x, ga_x, gb_x, w_pa, w_pb, w_out)
    if not with_ctx:
        return y_x, None
    o_na_c = _context_attention(qc, kc, vc)
    y_c = _merge(_gated_head_norm(o_dn_c, z_c, dn_norm_w), o_na_c, ga_c, gb_c, w_pa, w_pb, w_out)
    return y_x, y_c


def _peer_ffn(h, w_q, sub_keys, u_emb, v_emb):
    T, D = h.shape

    def block(hb):
        tb = hb.shape[0]
        q = (hb @ w_q).reshape(tb, PEER_HEADS, 2, PEER_DK)
        s = jnp.einsum('thpd,hpnd->thpn', q, sub_keys).astype(jnp.float32)
        top_s, top_i = lax.top_k(s, PEER_TOPK)
        cand_s = top_s[:, :, 0, :, None] + top_s[:, :, 1, None, :]
        cand_i = top_i[:, :, 0, :, None] * N_KEYS + top_i[:, :, 1, None, :]
        best_s, pos = lax.top_k(cand_s.reshape(tb, PEER_HEADS, PEER_TOPK * PEER_TOPK), PEER_TOPK)
        idx = jnp.take_along_axis(cand_i.reshape(tb, PEER_HEADS, PEER_TOPK * PEER_TOPK), pos, axis=-1)
        gate = jax.nn.softmax(best_s, axis=-1)
        act = jax.nn.gelu(jnp.einsum('td,thkd->thk', hb, u_emb[idx]))
        return jnp.einsum('thk,thkd->td', (gate * act).astype(hb.dtype), v_emb[idx])

    out = lax.map(block, h.reshape(T // PEER_BLOCK, PEER_BLOCK, D))
    return out.reshape(T, D)


def setup_inputs(seed: int = 0) -> dict:
    key = jax.random.key(seed)
    ks = jax.random.split(key, 24)
    f32 = jnp.float32

    def nrm(k, shape, s):
        return s * jax.random.normal(k, shape, f32)

    def gain(k, shape):
        return 1.0 + nrm(k, shape, 0.05)

    D = D_MODEL
    a_log = jnp.log(jax.random.uniform(ks[9], (DEPTH, 2, DN_HEADS), f32, A_MIN, A_MAX))
    dt = jnp.exp(jax.random.uniform(ks[10], (DEPTH, 2, DN_HEADS), f32,
                                    float(np.log(DT_MIN)), float(np.log(DT_MAX))))
    dt_bias = dt + jnp.log(-jnp.expm1(-dt))
    return {
        'x': nrm(ks[0], (BATCH, SEQ, D), 1.0),
        'c': nrm(ks[1], (BATCH, D), 1.0),
        'ctx': nrm(ks[2], (BATCH, CTX_LEN, D), 1.0),
        'c_ctx': nrm(ks[3], (D,), 1.0),
        'ada_w': nrm(ks[4], (DEPTH, D, N_MOD * D), 0.5 * D ** -0.5),
        'ada_b': nrm(ks[5], (DEPTH, N_MOD * D), 0.01),
        'norm1_w': gain(ks[6], (DEPTH, D)),
        'norm2_w': gain(ks[7], (DEPTH, D)),
        'w_in': nrm(ks[8], (DEPTH, D, IN_W), D ** -0.5),
        'dn_conv_w': nrm(ks[11], (DEPTH, CONV_K, 3 * DN_W), CONV_K ** -0.5),
        'dn_a_log': a_log,
        'dn_dt_bias': dt_bias,
        'dn_norm_w': gain(ks[12], (DEPTH, DN_DK)),
        'na_qnorm_w': gain(ks[13], (DEPTH, NA_DH)),
        'na_knorm_w': gain(ks[14], (DEPTH, NA_DH)),
        'na_rpb': nrm(ks[15], (DEPTH, NA_HEADS, 2 * WIN_R - 1, 2 * WIN_C - 1), 0.1),
        'w_pa': nrm(ks[16], (DEPTH, DN_W, D), DN_W ** -0.5),
        'w_pb': nrm(ks[17], (DEPTH, NA_W, D), NA_W ** -0.5),
        'w_out': nrm(ks[18], (DEPTH, D, D), D ** -0.5),
        'peer_wq': nrm(ks[19], (DEPTH, D, PEER_HEADS * 2 * PEER_DK), D ** -0.5),
        'peer_keys': nrm(ks[20], (DEPTH, PEER_HEADS, 2, N_KEYS, PEER_DK), PEER_DK ** -0.5),
        'peer_u': nrm(ks[21], (DEPTH, N_EXPERTS, D), D ** -0.5),
        'peer_v': nrm(ks[22], (DEPTH, N_EXPERTS, D), PEER_HEADS ** -0.5),
    }


def reference(x, c, ctx, c_ctx, ada_w, ada_b, norm1_w, norm2_w, w_in, dn_conv_w, dn_a_log,
              dn_dt_bias, dn_norm_w, na_qnorm_w, na_knorm_w, na_rpb, w_pa, w_pb, w_out,
              peer_wq, peer_keys, peer_u, peer_v):
    B, L, D = x.shape
    n_ctx = ctx.shape[1]
    for l in range(DEPTH):
        last = l == DEPTH - 1
        mod_x = jax.nn.silu(c) @ ada_w[l] + ada_b[l]
        mod_c = jax.nn.silu(c_ctx) @ ada_w[l] + ada_b[l]
        sh1x, sc1x, g1x, sh2x, sc2x, g2x = (t[:, None, :] for t in jnp.split(mod_x, N_MOD, axis=-1))
        sh1c, sc1c, g1c, sh2c, sc2c, g2c = jnp.split(mod_c, N_MOD, axis=-1)
        hx = _modulate(_rmsnorm(x, norm1_w[l]), sh1x, sc1x)
        hc = _modulate(_rmsnorm(ctx, norm1_w[l]), sh1c, sc1c)
        y_x, y_c = _hybrid_mixer(hx, hc, w_in[l], dn_conv_w[l], dn_a_log[l], dn_dt_bias[l],
                                 dn_norm_w[l], na_qnorm_w[l], na_knorm_w[l], na_rpb[l],
                                 w_pa[l], w_pb[l], w_out[l], not last)
        x = x + g1x * y_x
        hx = _modulate(_rmsnorm(x, norm2_w[l]), sh2x, sc2x)
        if last:
            f = _peer_ffn(hx.reshape(B * L, D), peer_wq[l], peer_keys[l], peer_u[l], peer_v[l])
            x = x + g2x * f.reshape(B, L, D)
        else:
            ctx = ctx + g1c * y_c
            hc = _modulate(_rmsnorm(ctx, norm2_w[l]), sh2c, sc2c)
            tokens = jnp.concatenate([hx.reshape(B * L, D), hc.reshape(B * n_ctx, D)], axis=0)
            f = _peer_ffn(tokens, peer_wq[l], peer_keys[l], peer_u[l], peer_v[l])
            x = x + g2x * f[:B * L].reshape(B, L, D)
            ctx = ctx + g2c * f[B * L:].reshape(B, n_ctx, D)
    return x
```

```python
import numpy as np
from contextlib import ExitStack, contextmanager
import concourse.bass as bass
import concourse.mybir as mybir
from concourse.bass_utils import run_bass_kernel_spmd

F32 = mybir.dt.float32
BF16 = mybir.dt.bfloat16
I32 = mybir.dt.int32
U32 = mybir.dt.uint32
AF = mybir.ActivationFunctionType
ALU = mybir.AluOpType
AX = mybir.AxisListType

D = 1024
LT = 4096
CT = 256
T = LT + CT
DEPTH = 4
IN_W = 5664
EPS = 1e-6
BLOCKS = [(i * 512, 512) for i in range(8)] + [(LT, CT)]
NTILE = T // 128


class Opnd:
    __slots__ = ("key", "ap")

    def __init__(self, key, ap):
        self.key = key
        self.ap = ap


class _Sub:
    def __init__(self, t, sub):
        self.t = t
        self.sub = sub

    def __getitem__(self, idx):
        return Opnd((self.t.name, self.sub), self.t.h[idx])


class Tile:
    def __init__(self, h, name):
        self.h = h
        self.name = name

    def __getitem__(self, idx):
        return Opnd(self.name, self.h[idx])

    def k(self, sub):
        return _Sub(self, sub)


def W(o, ap):
    return Opnd(o.key, ap)


class Prog:
    def __init__(self, nc, es):
        self.nc = nc
        self.es = es
        self.engs = {"pe": nc.tensor, "dve": nc.vector, "act": nc.scalar, "pool": nc.gpsimd, "sp": nc.sync}
        self.sem = {}
        self.cnt = {}
        for e in ("pe", "dve", "act", "pool"):
            self.sem[e] = es.enter_context(nc.semaphore("c_" + e))
            self.cnt[e] = 0
        self.dq = {}
        self.dptr = {}
        for q, n in (("sp", 32), ("pool", 24), ("act", 4)):
            ks = []
            for i in range(n):
                k = "d_%s%d" % (q, i)
                self.sem[k] = es.enter_context(nc.semaphore(k))
                self.cnt[k] = 0
                ks.append(k)
            self.dq[q] = ks
            self.dptr[q] = 0
        self.seen = {e: {} for e in self.engs}
        self.lw = {}
        self.rd = {}
        self.uid = 0
        self.ninst = 0
        self.psum_names = set()

    @contextmanager
    def sbuf(self, name, shape, dt):
        self.uid += 1
        nm = "%s_%d" % (name, self.uid)
        with self.nc.sbuf_tensor(nm, list(shape), dt) as h:
            yield Tile(h, nm)

    @contextmanager
    def psum(self, name, shape, dt):
        self.uid += 1
        nm = "%s_%d" % (name, self.uid)
        esz = 2 if dt == BF16 else 4
        n = 1
        for d_ in shape[1:]:
            n *= d_
        per_bank = 2048 // esz
        npad = ((n + per_bank - 1) // per_bank) * per_bank
        self.psum_names.add(nm)
        with self.nc.psum_tensor(nm, [shape[0], npad], dt) as h:
            v = h[:, 0:n]
            if len(shape) == 3:
                v = v.rearrange("p (a b) -> p a b", a=shape[1])
            elif len(shape) == 4:
                v = v.rearrange("p (a b c) -> p a b c", a=shape[1], b=shape[2])
            yield Tile(v, nm)

    def _deps(self, reads, writes):
        d = {}

        def add(tok):
            if tok is None:
                return
            k, v = tok
            if d.get(k, 0) < v:
                d[k] = v

        for r in reads:
            add(self.lw.get(r))
        for w in writes:
            add(self.lw.get(w))
            for k, v in self.rd.get(w, {}).items():
                add((k, v))
        return d

    def _wait(self, e, deps):
        eng = self.engs[e]
        seen = self.seen[e]
        for k, v in deps.items():
            if e == "pe" and k == "pe":
                continue
            if seen.get(k, 0) < v:
                eng.wait_ge(self.sem[k], v)
                seen[k] = v

    def _commit(self, tok, reads, writes):
        for w in writes:
            self.lw[w] = tok
            self.rd[w] = {}
        k, v = tok
        for r in reads:
            m = self.rd.setdefault(r, {})
            if m.get(k, 0) < v:
                m[k] = v

    def _is_psum(self, key):
        return (key if isinstance(key, str) else key[0]) in self.psum_names

    def op(self, e, fn, outs, ins):
        reads = [o.key for o in ins]
        writes = [o.key for o in outs]
        writes = writes + [k for k in reads if self._is_psum(k)]
        self._wait(e, self._deps(reads, writes))
        ins_ = fn(self.engs[e])
        self.cnt[e] += 1
        ins_.then_inc(self.sem[e], 1)
        self._commit((e, self.cnt[e]), reads, writes)
        self.ninst += 1

    def dma(self, q, out, in_, extra_reads=(), **kw):
        reads = [in_.key] + [o.key for o in extra_reads]
        writes = [out.key]
        ks = self.dq[q]
        k = ks[self.dptr[q] % len(ks)]
        self.dptr[q] += 1
        deps = self._deps(reads, writes)
        deps[k] = self.cnt[k]
        self._wait(q, deps)
        ins_ = self.engs[q].dma_start(out=out.ap, in_=in_.ap, **kw)
        self.cnt[k] += 16
        ins_.then_inc(self.sem[k], 16)
        self._commit((k, self.cnt[k]), reads, writes)
        self.ninst += 1

    def gather(self, out, table, idx, nrows):
        q = "pool"
        reads = [table.key, idx.key]
        writes = [out.key]
        ks = self.dq[q]
        k = ks[self.dptr[q] % len(ks)]
        self.dptr[q] += 1
        deps = self._deps(reads, writes)
        deps[k] = self.cnt[k]
        self._wait(q, deps)
        ins_ = self.nc.gpsimd.indirect_dma_start(
            out=out.ap, out_offset=None, in_=table.ap,
            in_offset=bass.IndirectOffsetOnAxis(ap=idx.ap, axis=0))
        self.cnt[k] += 16
        ins_.then_inc(self.sem[k], 16)
        self._commit((k, self.cnt[k]), reads, writes)
        self.ninst += 1

    def barrier(self):
        for e in self.engs:
            self._wait(e, dict(self.cnt))

    def mm(self, out, lhsT, rhs, start=True, stop=True):
        self.op("pe", lambda g: g.matmul(out.ap, lhsT.ap, rhs.ap, start=start, stop=stop), [out], [lhsT, rhs])

    def tr(self, out, in_, ident):
        self.op("pe", lambda g: g.transpose(out.ap, in_.ap, ident.ap), [out], [in_, ident])

    def act(self, out, in_, func, bias=None, scale=None, e="act", accum=None):
        kw = {}
        ins = [in_]
        if bias is not None:
            if isinstance(bias, Opnd):
                kw["bias"] = bias.ap
                ins.append(bias)
            else:
                kw["bias"] = bias
        if scale is not None:
            if isinstance(scale, Opnd):
                kw["scale"] = scale.ap
                ins.append(scale)
            else:
                kw["scale"] = scale
        outs = [out]
        if accum is not None:
            kw["accum_out"] = accum.ap
            outs.append(accum)
        self.op("act", lambda g: g.activation(out.ap, in_.ap, func, **kw), outs, ins)

    def tt(self, out, in0, in1, op, e="dve"):
        self.op(e, lambda g: g.tensor_tensor(out.ap, in0.ap, in1.ap, op), [out], [in0, in1])

    def ts(self, out, in0, s1, op0, s2=None, op1=None, e="dve"):
        ins = [in0]
        a1 = s1
        if isinstance(s1, Opnd):
            a1 = s1.ap
            ins.append(s1)
        a2 = s2
        if isinstance(s2, Opnd):
            a2 = s2.ap
            ins.append(s2)
        if op1 is None:
            self.op(e, lambda g: g.tensor_scalar(out.ap, in0.ap, a1, None, op0), [out], ins)
        else:
            self.op(e, lambda g: g.tensor_scalar(out.ap, in0.ap, a1, a2, op0, op1), [out], ins)

    def stt(self, out, in0, scalar, in1, op0, op1, e="dve"):
        ins = [in0, in1]
        a = scalar
        if isinstance(scalar, Opnd):
            a = scalar.ap
            ins.append(scalar)
        self.op(e, lambda g: g.scalar_tensor_tensor(out.ap, in0.ap, a, in1.ap, op0, op1), [out], ins)

    def rsq(self, out, in_, mult, add):
        self.act(out, in_, AF.Ln, bias=add, scale=mult)
        self.act(out, out, AF.Exp, scale=-0.5)

    def ttr(self, out, in0, in1, accum, op0=ALU.mult, op1=ALU.add):
        self.op("dve", lambda g: g.tensor_tensor_reduce(out.ap, in0.ap, in1.ap, 1.0, 0.0, op0, op1, accum.ap), [out, accum], [in0, in1])

    def cp(self, out, in_, e="dve"):
        self.op(e, lambda g: g.tensor_copy(out.ap, in_.ap), [out], [in_])

    def red(self, out, in_, op, axis=AX.X, e="dve"):
        self.op(e, lambda g: g.tensor_reduce(out.ap, in_.ap, axis, op), [out], [in_])

    def memset(self, out, val, e="dve"):
        self.op(e, lambda g: g.memset(out.ap, val), [out], [])


def _consts():
    c = {}
    c["IDF"] = np.eye(128, dtype=np.float32)
    c["ONESM"] = np.full((128, 128), 1.0 / 1024.0, np.float32)
    blk = np.zeros((128, 128), np.float32)
    blk[:64, :64] = 1
    blk[64:, 64:] = 1
    c["BLK"] = blk
    tri = np.zeros((128, 128), np.float32)
    i = np.arange(64)
    tri[:64, :64] = (i[:, None] <= i[None, :])
    tri[64:, 64:] = (i[:, None] >= i[None, :])
    c["TRI"] = tri
    c["STRI"] = tri - np.eye(128, dtype=np.float32)
    ind = np.zeros((128, 2), np.float32)
    ind[:64, 0] = 1
    ind[64:, 1] = 1
    c["IND"] = ind
    rot = np.zeros((128, 128), np.float32)
    for m in range(128):
        if m % 64 < 32:
            rot[m + 32, m] = -1.0
        else:
            rot[m - 32, m] = 1.0
    c["ROT"] = rot
    t = np.arange(LT)
    row = (t // 64).astype(np.float32)
    col = (t % 64).astype(np.float32)
    inv = (np.float32(10000.0) ** (-np.arange(16, dtype=np.float32) / np.float32(16))).astype(np.float32)
    ang = np.concatenate([row[:, None] * inv, col[:, None] * inv], axis=-1).astype(np.float32)
    cs = np.cos(ang).astype(np.float32).T
    sn = np.sin(ang).astype(np.float32).T
    c["COS"] = np.ascontiguousarray(np.tile(cs, (4, 1)))
    c["SIN"] = np.ascontiguousarray(np.tile(sn, (4, 1)))
    c["IOTA16"] = np.tile(np.arange(16, dtype=np.float32)[None, :], (128, 1))
    return c


def _na_bias_tables(rpb):
    H = rpb.shape[0]
    kc = np.arange(64)[:, None]
    qc = np.arange(64)[None, :]
    ws = np.clip(qc - 8, 0, 48)
    inwin = (kc >= ws) & (kc < ws + 16)
    off = np.clip(kc - qc + 15, 0, 30)
    out = np.full((2, 64, H, 16, 64), -30000.0, np.float32)
    combos = [(dr0, 1, 1) for dr0 in range(-7, 7)] + [(-5, 0, 1), (3, 1, 0)]
    for di, (dr0, v0, v1) in enumerate(combos):
        for a in range(2):
            dr = dr0 + a
            if dr < -7 or dr > 7 or not (v0, v1)[a]:
                continue
            g = rpb[:, dr + 7, :][:, off]
            g = np.where(inwin[None], g, np.float32(-30000.0))
            out[a, :, :, di, :] = g.transpose(1, 0, 2)
    return np.ascontiguousarray(out.reshape(128, H, 16, 64))


def na_sched(r):
    rs = min(max(r - 4, 0), 56)
    out = []
    for kr0 in range(rs - (rs % 2), rs + 8, 2):
        v0 = rs <= kr0 < rs + 8
        v1 = rs <= kr0 + 1 < rs + 8
        dr0 = kr0 - r
        if v0 and v1:
            tbl = dr0 + 7
        elif v1:
            assert dr0 == -5
            tbl = 14
        else:
            assert dr0 == 3 and v0
            tbl = 15
        out.append((kr0 // 2, tbl))
    return out


def _layer_small(inp, l):
    d = {}
    d["ADABT"] = np.ascontiguousarray(np.repeat(inp["ada_b"][l].reshape(48, 128).T[:, :, None], 2, axis=2))
    d["N1W"] = np.ascontiguousarray(np.repeat(inp["norm1_w"][l].reshape(8, 128).T[:, :, None], 2, axis=2))
    d["N2W"] = np.ascontiguousarray(np.repeat(inp["norm2_w"][l].reshape(8, 128).T[:, :, None], 2, axis=2))
    d["CONVW"] = np.ascontiguousarray(inp["dn_conv_w"][l].T.reshape(12, 128, 5).transpose(1, 0, 2))
    d["ALOG"] = np.ascontiguousarray(np.tile(inp["dn_a_log"][l].reshape(1, 16), (128, 1)))
    d["DTB"] = np.ascontiguousarray(np.tile(inp["dn_dt_bias"][l].reshape(1, 16), (128, 1)))
    d["DNW"] = np.ascontiguousarray(np.tile(inp["dn_norm_w"][l].reshape(1, 64), (128, 1)))
    qk = np.stack([np.tile(inp["na_qnorm_w"][l], 2), np.tile(inp["na_knorm_w"][l], 2)], axis=1)
    d["NAW"] = np.ascontiguousarray(qk)
    d["BIAST"] = _na_bias_tables(inp["na_rpb"][l])
    d["KEYST"] = np.ascontiguousarray(inp["peer_keys"][l].transpose(3, 0, 1, 2).reshape(128, 16, 128))
    return d


SMALL_SHAPES = {"ADABT": (128, 48, 2), "N1W": (128, 8, 2), "N2W": (128, 8, 2), "CONVW": (128, 12, 5), "ALOG": (128, 16),
                "DTB": (128, 16), "DNW": (128, 64), "NAW": (128, 2), "KEYST": (128, 16, 128)}
BIAST_SHAPE = (128, 8, 16, 64)
CONST_SHAPES = {"IDF": (128, 128), "ONESM": (128, 128), "BLK": (128, 128), "TRI": (128, 128), "STRI": (128, 128),
                "IND": (128, 2), "ROT": (128, 128), "COS": (128, LT), "SIN": (128, LT), "IOTA16": (128, 16)}
BIG_SHAPES = {"ada_w": (DEPTH, D, 6 * D), "w_in": (DEPTH, D, IN_W), "w_pa": (DEPTH, 512, D), "w_pb": (DEPTH, 512, D),
              "w_out": (DEPTH, D, D), "peer_wq": (DEPTH, D, 2048)}
for _l in range(DEPTH):
    BIG_SHAPES["peer_u%d" % _l] = (16384, D)
    BIG_SHAPES["peer_v%d" % _l] = (16384, D)


class Ctx:
    pass


def dram_in(nc, K, name, shape, dt=F32):
    h = nc.dram_tensor(name, list(shape), dt, kind="ExternalInput")
    t = Tile(h.ap(), name)
    setattr(K, name, t)
    return t


def dram_scratch(nc, K, name, shape, dt=F32, kind="Internal"):
    h = nc.dram_tensor(name, list(shape), dt, kind=kind)
    t = Tile(h.ap(), name)
    setattr(K, name, t)
    return t


def stage_mod(P, K, l):
    with ExitStack() as es:
        Wb = [es.enter_context(P.sbuf("adaw%d" % i, [128, 8, 512], F32)) for i in range(2)]
        ps = es.enter_context(P.psum("modps", [128, 96], F32))
        aw = K.ada_w.h[l].rearrange("(kc p) n -> p kc n", p=128)
        for blk in range(12):
            Wt = Wb[blk % 2]
            P.dma("sp", Wt[:, :, :], Opnd(("ada_w", l), aw[:, :, blk * 512:(blk + 1) * 512]))
            for n4 in range(4):
                n = blk * 4 + n4
                for kc in range(8):
                    P.mm(ps[:, 2 * n:2 * n + 2], Wt[:, kc, n4 * 128:(n4 + 1) * 128], K.SC[:, kc, :],
                         start=(kc == 0), stop=(kc == 7))
        sm = K.small
        for n in range(48):
            P.tt(K.MOD.k(n)[:, n, :], ps[:, 2 * n:2 * n + 2], sm["ADABT"][:, n, :], ALU.add)
        for fc in range(8):
            P.stt(K.A1[:, fc, :], K.MOD.k(8 + fc)[:, 8 + fc, :], 1.0, sm["N1W"][:, fc, :], ALU.add, ALU.mult)
            P.stt(K.A2[:, fc, :], K.MOD.k(32 + fc)[:, 32 + fc, :], 1.0, sm["N2W"][:, fc, :], ALU.add, ALU.mult)
        P.barrier()


def modv(K, seg, fc, j):
    n = seg * 8 + fc
    return K.MOD.k(n)[:, n, j:j + 1]


def stage_norm(P, K, A, seg_shift, hT):
    XTv = K.XT.h.rearrange("(fc p) t -> p fc t", p=128)
    with ExitStack() as es:
        XB = [es.enter_context(P.sbuf("xb%d" % i, [128, 8, 512], F32)) for i in range(2)]
        SQ = es.enter_context(P.sbuf("sq", [128, 8, 512], F32))
        RS = es.enter_context(P.sbuf("rstd", [128, 512], F32))
        TMP = [es.enter_context(P.sbuf("ntmp%d" % i, [128, 512], F32)) for i in range(2)]
        ps = es.enter_context(P.psum("nps", [128, 512], F32))
        for bi, (t0, n) in enumerate(BLOCKS):
            j = 1 if bi == 8 else 0
            xb = XB[bi % 2]
            P.dma("sp", xb[:, :, :n], Opnd(("XT", bi), XTv[:, :, t0:t0 + n]))
            P.act(SQ[:, :, :n], xb[:, :, :n], AF.Square)
            for fc in range(8):
                P.mm(ps[:, :n], K.c["ONESM"][:, :], SQ[:, fc, :n], start=(fc == 0), stop=(fc == 7))
            P.rsq(RS[:, :n], ps[:, :n], 1.0, EPS)
            for fc in range(8):
                tmp = TMP[fc % 2]
                P.stt(tmp[:, :n], xb[:, fc, :n], A[:, fc, j:j + 1], RS[:, :n], ALU.mult, ALU.mult)
                P.act(hT.k(bi)[:, fc, t0:t0 + n], tmp[:, :n], AF.Identity, bias=modv(K, seg_shift, fc, j))
        P.barrier()


C_DQKV, C_Z, C_AB, C_NQ, C_NK, C_NV, C_GA, C_GB = 0, 1536, 2048, 2080, 2592, 3104, 3616, 4640
RAWLEN = LT + CT + 8
RAW_L0 = 2
RAW_C0 = LT + 6


def load_w_bf16(P, K, wsrc_key, wview, WS, WB, ncols):
    nk = WB.h.shape[1]
    P.dma("sp", WS[:, :nk, :ncols], Opnd(wsrc_key, wview))
    P.cp(WB[:, :, :ncols], WS[:, :nk, :ncols], e="pool")


def stage_inproj(P, K, l, hT):
    win = K.w_in.h[l].rearrange("(kc p) n -> p kc n", p=128)
    sm = K.small
    c = K.c
    with ExitStack() as es:
        WS = es.enter_context(P.sbuf("ws_t", [128, 8, 512], F32))
        WZ = es.enter_context(P.sbuf("wz", [128, 8, 512], BF16))
        WV = es.enter_context(P.sbuf("wv", [128, 8, 512], BF16))
        WA = es.enter_context(P.sbuf("wa", [128, 8, 32], BF16))
        NEGA = es.enter_context(P.sbuf("nega", [128, 16], F32))
        load_w_bf16(P, K, ("w_in", l), win[:, :, C_Z:C_Z + 512], WS, WZ, 512)
        load_w_bf16(P, K, ("w_in", l), win[:, :, C_NV:C_NV + 512], WS, WV, 512)
        load_w_bf16(P, K, ("w_in", l), win[:, :, C_AB:C_AB + 32], WS, WA, 32)
        P.act(NEGA[:, :], sm["ALOG"][:, :], AF.Exp)
        P.ts(NEGA[:, :], NEGA[:, :], -1.0, ALU.mult)
        PZ = [es.enter_context(P.psum("pz%d" % i, [128, 512], F32)) for i in range(2)]
        PV = [es.enter_context(P.psum("pv%d" % i, [128, 512], F32)) for i in range(2)]
        PA = [es.enter_context(P.psum("pa%d" % i, [128, 32], F32)) for i in range(2)]
        ZO = [es.enter_context(P.sbuf("zo%d" % i, [128, 512], F32)) for i in range(2)]
        VO = [es.enter_context(P.sbuf("vo%d" % i, [128, 512], BF16)) for i in range(2)]
        GO = [es.enter_context(P.sbuf("go%d" % i, [128, 32], F32)) for i in range(2)]
        GT = [es.enter_context(P.sbuf("gt%d" % i, [128, 16], F32)) for i in range(2)]
        for ti in range(NTILE):
            t0 = ti * 128
            bi = min(t0 // 512, 8)
            pz, pv, pa = PZ[ti % 2], PV[ti % 2], PA[ti % 2]
            zo, vo, go, gt = ZO[ti % 2], VO[ti % 2], GO[ti % 2], GT[ti % 2]
            for kc in range(8):
                P.mm(pz[:, :], hT.k(bi)[:, kc, t0:t0 + 128], WZ[:, kc, :], start=(kc == 0), stop=(kc == 7))
            for kc in range(8):
                P.mm(pv[:, :], hT.k(bi)[:, kc, t0:t0 + 128], WV[:, kc, :], start=(kc == 0), stop=(kc == 7))
            for kc in range(8):
                P.mm(pa[:, :], hT.k(bi)[:, kc, t0:t0 + 128], WA[:, kc, :], start=(kc == 0), stop=(kc == 7))
            P.act(zo[:, :], pz[:, :], AF.Silu)
            P.dma("sp", Opnd(("ZS", ti), K.ZS.h[t0:t0 + 128, :]), zo[:, :])
            P.cp(vo[:, :], pv[:, :])
            P.dma("sp", Opnd(("NV", ti), K.NV.h[t0:t0 + 128, :]), vo[:, :])
            P.tt(gt[:, :], pa[:, 0:16], sm["DTB"][:, :], ALU.add)
            P.act(gt[:, :], gt[:, :], AF.Exp)
            P.act(gt[:, :], gt[:, :], AF.Ln, bias=1.0)
            P.tt(go[:, 0:16], gt[:, :], NEGA[:, :], ALU.mult)
            P.act(go[:, 16:32], pa[:, 16:32], AF.Sigmoid)
            P.dma("sp", Opnd(("GB", ti), K.GB.h[t0:t0 + 128, :]), go[:, :])
        P.barrier()
    with ExitStack() as es:
        WS = [es.enter_context(P.sbuf("ws%d" % i, [128, 8, 128], F32)) for i in range(2)]
        WB = [es.enter_context(P.sbuf("wb%d" % i, [128, 8, 128], BF16)) for i in range(2)]
        PS = [es.enter_context(P.psum("ips%d" % i, [128, 512], F32)) for i in range(2)]
        PS2 = [es.enter_context(P.psum("ips2%d" % i, [128, 512], F32)) for i in range(2)]
        RAW = es.enter_context(P.sbuf("raw", [128, RAWLEN], F32))
        Y = es.enter_context(P.sbuf("convy", [128, T], F32))
        COS = es.enter_context(P.sbuf("cos", [128, LT], F32))
        SIN = es.enter_context(P.sbuf("sin", [128, LT], F32))
        T1 = [es.enter_context(P.sbuf("rt1%d" % i, [128, 512], F32)) for i in range(2)]
        T2 = [es.enter_context(P.sbuf("rt2%d" % i, [128, 512], F32)) for i in range(2)]
        T3 = [es.enter_context(P.sbuf("rt3%d" % i, [128, 512], F32)) for i in range(2)]
        OB = [es.enter_context(P.sbuf("rob%d" % i, [128, 512], BF16)) for i in range(2)]
        P.dma("sp", COS[:, :], K.dCOS[:, :])
        P.dma("sp", SIN[:, :], K.dSIN[:, :])
        P.memset(RAW[:, :], 0.0)
        cnt = [0]

        def chunk_mm(col0, bi):
            pass

        nchunk = 0
        for ci in range(12 + 8 + 16):
            if ci < 12:
                col0 = C_DQKV + ci * 128
            elif ci < 20:
                col0 = C_NQ + (ci - 12) * 128
            else:
                col0 = C_GA + (ci - 20) * 128
            ws, wb = WS[ci % 2], WB[ci % 2]
            load_w_bf16(P, K, ("w_in", l), win[:, :, col0:col0 + 128], ws, wb, 128)
            for bi, (t0, n) in enumerate(BLOCKS):
                cnt[0] += 1
                u = cnt[0] % 2
                ps = PS[u]
                for kc in range(8):
                    P.mm(ps[:, :n], wb[:, kc, :], hT.k(bi)[:, kc, t0:t0 + n], start=(kc == 0), stop=(kc == 7))
                if ci < 12:
                    r0 = RAW_L0 + t0 if bi < 8 else RAW_C0
                    P.cp(RAW.k(bi)[:, r0:r0 + n], ps[:, :n], e="act") if False else P.act(RAW.k(bi)[:, r0:r0 + n], ps[:, :n], AF.Copy)
                elif ci < 20:
                    isq = ci < 16
                    t1, t2, t3, ob, ps2 = T1[u], T2[u], T3[u], OB[u], PS2[u]
                    P.act(t1[:, :n], ps[:, :n], AF.Copy)
                    P.act(t2[:, :n], ps[:, :n], AF.Square)
                    P.mm(ps2[:, :n], c["BLK"][:, :], t2[:, :n])
                    P.rsq(t3[:, :n], ps2[:, :n], 1.0 / 64.0, EPS)
                    P.stt(ob[:, :n], t1[:, :n], sm["NAW"][:, (0 if isq else 1):(1 if isq else 2)], t3[:, :n], ALU.mult, ALU.mult)
                    if isq:
                        P.ts(ob[:, :n], ob[:, :n], 0.125, ALU.mult)
                    dst = K.NQT if isq else K.NKT
                    r = ((ci - 12) % 4) * 128
                    P.dma("sp", Opnd((dst.name, ci, bi), dst.h[r:r + 128, t0:t0 + n]), ob[:, :n])
                else:
                    t1 = T1[u]
                    P.act(t1[:, :n], ps[:, :n], AF.Sigmoid)
                    r = (ci - 20) * 128
                    P.dma("sp", Opnd(("SG", ci, bi), K.SG.h[r:r + 128, t0:t0 + n]), t1[:, :n])
            if ci < 12:
                cw = sm["CONVW"]
                for (y0, r0, n) in ((0, RAW_L0 - 2, LT), (LT, RAW_C0 - 2, CT)):
                    rr = [RAW.k(b) for b in range(9)]
                    ins_all = [RAW.k(b)[:, 0:1] for b in range(9)]
                    for j in range(5):
                        src = Opnd(("RAWALL",), RAW.h[:, r0 + j:r0 + j + n])
                        if j == 0:
                            P.op("dve", lambda g, s=src, j=j: g.tensor_scalar(Y.h[:, y0:y0 + n], s.ap, cw.h[:, ci, j:j + 1], None, ALU.mult),
                                 [Y.k(y0)[:, y0:y0 + n]], ins_all + [cw[:, ci, :]])
                        else:
                            P.op("dve", lambda g, s=src, j=j: g.scalar_tensor_tensor(Y.h[:, y0:y0 + n], s.ap, cw.h[:, ci, j:j + 1], Y.h[:, y0:y0 + n], ALU.mult, ALU.add),
                                 [Y.k(y0)[:, y0:y0 + n]], ins_all + [cw[:, ci, :], Y.k(y0)[:, y0:y0 + n]])
                    P.act(Y.k(y0)[:, y0:y0 + n], Y.k(y0)[:, y0:y0 + n], AF.Silu)
                if ci < 8:
                    for bi, (t0, n) in enumerate(BLOCKS):
                        cnt[0] += 1
                        u = cnt[0] % 2
                        yk = Y.k(0 if bi < 8 else LT)
                        t1, t2, t3, ps2, ps = T1[u], T2[u], T3[u], PS2[u], PS[u]
                        P.act(t1[:, :n], yk[:, t0:t0 + n], AF.Square)
                        P.mm(ps2[:, :n], c["BLK"][:, :], t1[:, :n])
                        P.rsq(t2[:, :n], ps2[:, :n], 1.0, EPS)
                        if bi < 8:
                            P.tt(t3[:, :n], yk[:, t0:t0 + n], t2[:, :n], ALU.mult)
                            P.mm(ps[:, :n], c["ROT"][:, :], t3[:, :n])
                            P.tt(t1[:, :n], ps[:, :n], SIN[:, t0:t0 + n], ALU.mult)
                            P.tt(t3[:, :n], t3[:, :n], COS[:, t0:t0 + n], ALU.mult)
                            P.tt(t3[:, :n], t3[:, :n], t1[:, :n], ALU.add)
                        else:
                            P.tt(t3[:, :n], yk[:, t0:t0 + n], t2[:, :n], ALU.mult)
                        P.dma("sp", Opnd(("DQKVT", ci, bi), K.DQKVT.h[ci * 128:(ci + 1) * 128, t0:t0 + n]), t3[:, :n])
                else:
                    P.dma("sp", Opnd(("DQKVT", ci, 0), K.DQKVT.h[ci * 128:(ci + 1) * 128, 0:LT]), Y.k(0)[:, 0:LT])
                    P.dma("sp", Opnd(("DQKVT", ci, 8), K.DQKVT.h[ci * 128:(ci + 1) * 128, LT:T]), Y.k(LT)[:, LT:T])
        P.barrier()


SCRATCH = {"XT": ((D, T), F32), "ZS": ((T, 512), F32), "NV": ((T, 512), BF16), "GB": ((T, 32), F32),
           "DQKVT": ((1536, T), F32), "NQT": ((512, T), BF16), "NKT": ((512, T), BF16), "SG": ((2048, T), F32),
           "ODN": ((2, T, 512), F32), "OBT": ((512, T), BF16), "FT": ((T, D), F32)}


def build(nlayers=DEPTH, stop_after=None, debug=()):
    nc = bass.Bass("TRN2", target_bir_lowering=False)
    K = Ctx()
    dram_in(nc, K, "XT0", (D, T))
    dram_in(nc, K, "CIN", (128, 8, 2))
    for n, shp in BIG_SHAPES.items():
        dram_in(nc, K, n, shp)
    dconst = {n: dram_in(nc, K, "c_" + n, shp) for n, shp in CONST_SHAPES.items()}
    dsmall = [{n: dram_in(nc, K, "s%d_%s" % (l, n), shp) for n, shp in SMALL_SHAPES.items()} for l in range(DEPTH)]
    K.dBIAST = [dram_in(nc, K, "s%d_BIAST" % l, BIAST_SHAPE) for l in range(DEPTH)]
    K.dCOS, K.dSIN = dconst["COS"], dconst["SIN"]
    for n, (shp, dt) in SCRATCH.items():
        dram_scratch(nc, K, n, shp, dt, kind=("ExternalOutput" if n in debug else "Internal"))
    dram_scratch(nc, K, "YT", (D, LT), F32, kind="ExternalOutput")
    with ExitStack() as es:
        P = Prog(nc, es)
        K.c = {}
        for n, shp in CONST_SHAPES.items():
            if n in ("COS", "SIN"):
                continue
            K.c[n] = es.enter_context(P.sbuf("k" + n, shp, F32))
            P.dma("sp", K.c[n][tuple(slice(None) for _ in shp)], dconst[n][tuple(slice(None) for _ in shp)])
        K.small = {n: es.enter_context(P.sbuf("sm" + n, shp, F32)) for n, shp in SMALL_SHAPES.items()}
        K.SC = es.enter_context(P.sbuf("SC", [128, 8, 2], F32))
        K.MOD = es.enter_context(P.sbuf("MOD", [128, 48, 2], F32))
        K.A1 = es.enter_context(P.sbuf("A1", [128, 8, 2], F32))
        K.A2 = es.enter_context(P.sbuf("A2", [128, 8, 2], F32))
        P.dma("sp", K.SC[:, :, :], K.CIN[:, :, :])
        P.act(K.SC[:, :, :], K.SC[:, :, :], AF.Silu)
        for bi, (t0, n) in enumerate(BLOCKS):
            P.dma("sp", Opnd(("XT", bi), K.XT.h[:, t0:t0 + n]), Opnd("XT0", K.XT0.h[:, t0:t0 + n]))

        def stop(tag):
            return stop_after == tag

        for l in range(nlayers):
            for n, shp in SMALL_SHAPES.items():
                sl = tuple(slice(None) for _ in shp)
                P.dma("sp", K.small[n][sl], dsmall[l][n][sl])
            stage_mod(P, K, l)
            if stop("mod"):
                break
            with P.sbuf("hT", [128, 8, T], BF16) as hT:
                stage_norm(P, K, K.A1, 0, hT)
                stage_inproj(P, K, l, hT)
                P.barrier()
            if stop("inproj"):
                break
            if "skipdn" not in debug:
                stage_dn(P, K)
            if stop("dn"):
                break
            stage_na(P, K, l)
            if stop("na"):
                break
            stage_merge(P, K, l)
            if stop("merge"):
                break
            stage_peer(P, K, l, tiles=getattr(K, "peer_tiles", None))
            if stop("peer"):
                break
        P.barrier()
        if "MODOUT" in debug:
            dram_scratch(nc, K, "MODOUT", (128, 48, 2), F32, kind="ExternalOutput")
            P.dma("sp", K.MODOUT[:, :, :], K.MOD[:, :, :])
        for bi in range(8):
            t0 = bi * 512
            P.dma("sp", Opnd(("YT", bi), K.YT.h[:, t0:t0 + 512]), Opnd(("XT", bi), K.XT.h[:, t0:t0 + 512]))
        P.barrier()
        print("instructions:", P.ninst, flush=True)
    return nc


def make_in_maps(inputs):
    inp = {k: np.asarray(v) for k, v in inputs.items()}
    consts = _consts()
    shared = {}
    for n in BIG_SHAPES:
        if n.startswith("peer_u") or n.startswith("peer_v"):
            shared[n] = np.ascontiguousarray(inp[n[:6]][int(n[6:])], dtype=np.float32)
        else:
            shared[n] = np.ascontiguousarray(inp[n], dtype=np.float32)
    for n, v in consts.items():
        shared["c_" + n] = np.ascontiguousarray(v, dtype=np.float32)
    for l in range(DEPTH):
        for n, v in _layer_small(inp, l).items():
            shared["s%d_%s" % (l, n)] = np.ascontiguousarray(v, dtype=np.float32)
    maps = []
    for b in range(8):
        m = dict(shared)
        xt = np.concatenate([inp["x"][b].T, inp["ctx"][b].T], axis=1)
        m["XT0"] = np.ascontiguousarray(xt, dtype=np.float32)
        cin = np.stack([inp["c"][b].reshape(8, 128).T, inp["c_ctx"].reshape(8, 128).T], axis=2)
        m["CIN"] = np.ascontiguousarray(cin, dtype=np.float32)
        maps.append(m)
    return maps


def kernel(**inputs):
    nc = build()
    maps = make_in_maps(inputs)
    res = run_bass_kernel_spmd(nc, maps, core_ids=list(range(8)))
    out = np.stack([np.ascontiguousarray(res.results[b]["YT"].T) for b in range(8)], axis=0)
    return out.astype(np.float32)


def bc(o, axis, shape):
    return Opnd(o.key, o.ap.unsqueeze(axis).to_broadcast(list(shape)))


def stage_dn(P, K):
    c = K.c
    steps = [(LT, 4, i) for i in range(4)] + [(0, 64, i) for i in range(64)]
    DQ = K.DQKVT.h
    GBv = K.GB.h.rearrange("t (a d h) -> t a d h", a=2, d=2)
    SH = [128, 8, 128]
    with ExitStack() as es:
        def sb(n, s=SH, dt=F32):
            return es.enter_context(P.sbuf(n, s, dt))
        KBD = [sb("kbd%d" % i) for i in range(2)]
        QBD = [sb("qbd%d" % i) for i in range(2)]
        VT2 = [sb("vt2%d" % i, [64, 8, 128]) for i in range(2)]
        GBt = [sb("gbt%d" % i, [128, 2, 8]) for i in range(2)]
        X = [es.enter_context(P.psum("dx%d" % i, [128, 1024], F32)) for i in range(4)]
        S = sb("S", [128, 8, 64])
        GC = sb("GC", [128, 16])
        EG = sb("EG", [128, 8])
        EGL = sb("EGL", [128, 8])
        EDGL = sb("EDGL", [128, 8])
        GL, DT, DECI, DECS, EGB, XM, XMT, INTRAT, QD, KG, KDEC, R, UT = [sb(n) for n in (
            "GL", "DT", "DECI", "DECS", "EGB", "XM", "XMT", "INTRAT", "QD", "KG", "KDEC", "R", "UT")]
        Yb = [sb("Y%d" % i) for i in range(2)]
        YTb = [sb("YT%d" % i) for i in range(2)]
        VS, BW, T1, VN, OO = [sb(n, [128, 8, 64]) for n in ("VS", "BW", "T1", "VN", "OO")]
        for t_ in KBD + QBD:
            P.memset(t_[:, :, :], 0.0, e="pool")
        P.memset(S[:, :, :], 0.0)
        for si, (base, nch, i) in enumerate(steps):
            u = si % 2
            tf = base + 64 * i
            tb = base + 64 * (nch - 1 - i)
            kbd, qbd, vt2, gbt = KBD[u], QBD[u], VT2[u], GBt[u]
            for (half, t0) in ((0, tf), (1, tb)):
                ps_ = slice(64 * half, 64 * half + 64)
                P.dma("sp", kbd[ps_, :, ps_], Opnd("DQKVT", DQ[512:1024, t0:t0 + 64].rearrange("(h d) t -> d h t", h=8)))
                P.dma("sp", qbd[ps_, :, ps_], Opnd("DQKVT", DQ[0:512, t0:t0 + 64].rearrange("(h d) t -> d h t", h=8)))
                P.dma("sp", vt2[:, :, ps_], Opnd("DQKVT", DQ[1024:1536, t0:t0 + 64].rearrange("(h d) t -> d h t", h=8)))
                P.dma("sp", gbt[ps_, :, :], Opnd("GB", GBv[t0:t0 + 64, :, half, :]))
            gs = gbt[:, 0, :]
            be = gbt[:, 1, :]
            P.mm(X[0][:, 0:8], c["TRI"][:, :], gs)
            P.mm(X[0][:, 8:16], c["BLK"][:, :], gs)
            P.cp(GC[:, :], X[0][:, 0:16])
            P.act(EG[:, :], GC[:, 0:8], AF.Exp)
            P.act(EGL[:, :], GC[:, 8:16], AF.Exp)
            P.tt(EDGL[:, :], GC[:, 8:16], GC[:, 0:8], ALU.subtract)
            P.act(EDGL[:, :], EDGL[:, :], AF.Exp)
            P.tt(GL[:, :, :], bc(c["BLK"][:, :], 1, SH), bc(gs, 2, SH), ALU.mult)
            x1 = X[1][:, :].ap.rearrange("p (h c) -> p h c", h=8)
            X1 = Opnd(X[1].name, x1)
            for h in range(8):
                P.mm(Opnd(X[1].name, x1[:, h, :]), GL[:, h, :], c["TRI"][:, :])
            P.tt(DT[:, :, :], X1, bc(GC[:, 0:8], 2, SH), ALU.subtract)
            P.ts(DT[:, :, :], DT[:, :, :], 0.0, ALU.min)
            P.act(DT[:, :, :], DT[:, :, :], AF.Exp)
            P.tt(DECI[:, :, :], DT[:, :, :], bc(c["TRI"][:, :], 1, SH), ALU.mult, e="pool")
            P.tt(DECS[:, :, :], DT[:, :, :], bc(c["STRI"][:, :], 1, SH), ALU.mult)
            P.act(EGB[:, :, :], X1, AF.Exp)
            x2 = X[2][:, :].ap.rearrange("p (h c) -> p h c", h=8)
            x3 = X[3][:, :].ap.rearrange("p (h c) -> p h c", h=8)
            x0 = X[0][:, :].ap.rearrange("p (h c) -> p h c", h=8)
            X0, X2, X3 = Opnd(X[0].name, x0), Opnd(X[2].name, x2), Opnd(X[3].name, x3)
            for h in range(8):
                P.mm(Opnd(X[2].name, x2[:, h, :]), kbd[:, h, :], kbd[:, h, :])
            for h in range(8):
                P.mm(Opnd(X[3].name, x3[:, h, :]), kbd[:, h, :], qbd[:, h, :])
            P.tt(XM[:, :, :], X2, DECS[:, :, :], ALU.mult)
            P.tt(XM[:, :, :], XM[:, :, :], bc(be, 2, SH), ALU.mult)
            P.stt(INTRAT[:, :, :], X3, 0.125, DECI[:, :, :], ALU.mult, ALU.mult)
            P.stt(QD[:, :, :], qbd[:, :, :], 0.125, EGB[:, :, :], ALU.mult, ALU.mult)
            for h in range(8):
                P.tr(Opnd(X[0].name, x0[:, h, :]), kbd[:, h, :], c["IDF"][:, :])
            P.tt(KG[:, :, :], X0, bc(EG[:, :], 2, SH), ALU.mult)
            P.tt(KDEC[:, :, :], X0, bc(EDGL[:, :], 2, SH), ALU.mult)
            x1v = X[1][:, 0:512].ap.rearrange("p (h c) -> p h c", h=8)
            for h in range(8):
                P.tr(Opnd(X[1].name, x1v[:, h, :]), vt2[:, h, :], c["IDF"][0:64, 0:64])
            P.act(VS[:, :, :], Opnd(X[1].name, x1v), AF.Copy)
            for h in range(8):
                P.tr(Opnd(X[2].name, x2[:, h, :]), XM[:, h, :], c["IDF"][:, :])
            P.act(XMT[:, :, :], X2, AF.Copy)
            P.tt(R[:, :, :], bc(c["IDF"][:, :], 1, SH), XM[:, :, :], ALU.subtract)
            yp, ytp = XM, XMT
            for lv in range(5):
                yn, ytn = Yb[lv % 2], YTb[lv % 2]
                last = lv == 4
                if not last:
                    for h in range(8):
                        P.mm(Opnd(X[3].name, x3[:, h, :]), ytp[:, h, :], yp[:, h, :])
                    P.act(yn[:, :, :], X3, AF.Copy)
                for h in range(8):
                    P.mm(Opnd(X[0].name, x0[:, h, :]), yp[:, h, :], ytp[:, h, :])
                P.cp(ytn[:, :, :], X0)
                for h in range(8):
                    P.mm(Opnd(X[2].name, x2[:, h, :]), ytn[:, h, :], R[:, h, :])
                P.tt(R[:, :, :], R[:, :, :], X2, ALU.add)
                yp, ytp = yn, ytn
            for h in range(8):
                P.mm(Opnd(X[3].name, x3[:, h, :]), KG[:, h, :], R[:, h, :])
            P.act(UT[:, :, :], X3, AF.Copy)
            for h in range(8):
                P.mm(Opnd(X[1].name, x1v[:, h, :]), R[:, h, :], VS[:, h, :])
            S64 = [128, 8, 64]
            P.tt(BW[:, :, :], Opnd(X[1].name, x1v), bc(be, 2, S64), ALU.mult)
            x0v = X[0][:, 0:512].ap.rearrange("p (h c) -> p h c", h=8)
            x2v = X[2][:, 0:512].ap.rearrange("p (h c) -> p h c", h=8)
            x3v = X[3][:, 0:512].ap.rearrange("p (h c) -> p h c", h=8)
            for h in range(8):
                P.mm(Opnd(X[0].name, x0v[:, h, :]), UT[:, h, :], S[:, h, :])
            P.tt(T1[:, :, :], Opnd(X[0].name, x0v), bc(be, 2, S64), ALU.mult)
            P.tt(VN[:, :, :], BW[:, :, :], T1[:, :, :], ALU.subtract)
            for h in range(8):
                P.mm(Opnd(X[2].name, x2v[:, h, :]), QD[:, h, :], S[:, h, :], start=True, stop=False)
                P.mm(Opnd(X[2].name, x2v[:, h, :]), INTRAT[:, h, :], VN[:, h, :], start=False, stop=True)
            P.act(OO[:, :, :], Opnd(X[2].name, x2v), AF.Copy)
            for h in range(8):
                P.mm(Opnd(X[3].name, x3v[:, h, :]), KDEC[:, h, :], VN[:, h, :])
            P.tt(S[:, :, :], S[:, :, :], bc(EGL[:, :], 2, S64), ALU.mult)
            P.tt(S[:, :, :], S[:, :, :], Opnd(X[3].name, x3v), ALU.add)
            oov = OO.h[:, :, :].rearrange("p h e -> p (h e)")
            P.dma("sp", Opnd(("ODN", 0, tf), K.ODN.h[0, tf:tf + 64, :]), Opnd(OO.name, oov[0:64, :]))
            P.dma("sp", Opnd(("ODN", 1, tb), K.ODN.h[1, tb:tb + 64, :]), Opnd(OO.name, oov[64:128, :]))
        P.barrier()


def stage_na(P, K, l):
    c = K.c
    with ExitStack() as es:
        def sb(n, s, dt=F32):
            return es.enter_context(P.sbuf(n, s, dt))
        NKT = sb("nkt", [128, 4, T], BF16)
        NQT = sb("nqt", [128, 4, T], BF16)
        NV = sb("nv", [128, NTILE, 512], BF16)
        OBT = sb("obt", [128, 4, T], BF16)
        BIASB = sb("biasb", [128, 8, 16, 64], BF16)
        IDB = sb("idb", [128, 128], BF16)
        ONESB = sb("onesb", [128, 128], BF16)
        PT = [sb("pt%d" % i, [128, 7, 64], BF16) for i in range(2)]
        RD = [sb("rd%d" % i, [128, 64], F32) for i in range(2)]
        PS = [es.enter_context(P.psum("nps%d" % i, [128, 7, 64], F32)) for i in range(2)]
        PO = [es.enter_context(P.psum("npo%d" % i, [128, 64], F32)) for i in range(2)]
        PD = [es.enter_context(P.psum("npd%d" % i, [128, 64], F32)) for i in range(2)]
        for cch in range(4):
            P.dma("sp", NKT.k(cch)[:, cch, :], Opnd("NKT", K.NKT.h[cch * 128:(cch + 1) * 128, :]))
            P.dma("sp", NQT.k(cch)[:, cch, :], Opnd("NQT", K.NQT.h[cch * 128:(cch + 1) * 128, :]))
        nvv = K.NV.h.rearrange("(n p) f -> p n f", p=128)
        for g4 in range(0, NTILE, 2):
            P.dma("sp", NV.k(g4)[:, g4:g4 + 2, :], Opnd("NV", nvv[:, g4:g4 + 2, :]))
        with P.sbuf("biasf", [128, 4, 16, 64], F32) as BIASF:
            for hh in range(2):
                P.dma("sp", BIASF[:, :, :, :], Opnd(("BIAST", l), K.dBIAST[l].h[:, hh * 4:(hh + 1) * 4, :, :]))
                P.cp(BIASB[:, hh * 4:(hh + 1) * 4, :, :], BIASF[:, :, :, :], e="pool")
            P.barrier()
        P.cp(IDB[:, :], c["IDF"][:, :])
        P.memset(ONESB[:, :], 1.0)
        if hasattr(K, "na_rows"):
            P.memset(OBT[:, :, :], 0.0, e="pool")
            P.barrier()
        it = 0
        for r in getattr(K, 'na_rows', range(68)):
            if r < 64:
                loc = na_sched(r)
                chunks = [(kt, tbl) for kt, tbl in loc] + [(32, None), (33, None)]
                q0 = r * 64
            else:
                chunks = [(32, None), (33, None)]
                q0 = LT + (r - 64) * 64
            nch = len(chunks)
            for h in range(8):
                hc, hp = h // 2, h % 2
                pr = slice(64 * hp, 64 * hp + 64)
                u = it % 2
                it += 1
                ps, po, pd, pt, rd = PS[u], PO[u], PD[u], PT[u], RD[u]
                for j, (kt, tbl) in enumerate(chunks):
                    has_b = tbl is not None
                    P.mm(ps.k(j)[:, j, :], NKT.k(hc)[pr, hc, kt * 128:(kt + 1) * 128], NQT.k(hc)[pr, hc, q0:q0 + 64],
                         start=True, stop=not has_b)
                    if has_b:
                        P.mm(ps.k(j)[:, j, :], IDB[:, :], BIASB[:, h, tbl, :], start=False, stop=True)
                psall = Opnd(ps.name, ps.h[:, 0:nch, :])
                P.op("act", lambda g, a=pt.h[:, 0:nch, :], b=ps.h[:, 0:nch, :]: g.activation(a, b, AF.Exp),
                     [pt[:, :, :]], [ps.k(j)[:, j, :] for j in range(nch)])
                for j, (kt, tbl) in enumerate(chunks):
                    g4 = kt - (kt % 2)
                    P.mm(po[:, :], NV.k(g4)[:, kt, hc * 128:(hc + 1) * 128], pt[:, j, :], start=(j == 0), stop=(j == nch - 1))
                for j, (kt, tbl) in enumerate(chunks):
                    P.mm(pd[:, :], ONESB[:, :], pt[:, j, :], start=(j == 0), stop=(j == nch - 1))
                P.op("dve", lambda g, a=rd.h[pr, :], b=pd.h[pr, :]: g.reciprocal(a, b), [rd[:, :]], [pd[:, :]])
                P.op("dve", lambda g, a=OBT.h[pr, hc, q0:q0 + 64], b=po.h[pr, :], c_=rd.h[pr, :]: g.tensor_tensor(a, b, c_, ALU.mult),
                     [OBT.k((r, h))[:, 0, 0:1]], [po[:, :], rd[:, :]])
        P.barrier()
        for cch in range(4):
            P.dma("sp", Opnd("OBTd", K.OBT.h[cch * 128:(cch + 1) * 128, :]), Opnd(OBT.name, OBT.h[:, cch, :]))
        P.barrier()


def stage_merge(P, K, l):
    c = K.c
    sm = K.small
    XTv = K.XT.h.rearrange("(fc p) t -> p fc t", p=128)
    SGv = K.SG.h.rearrange("(g n p) t -> p g n t", g=2, p=128)
    OBv = K.OBT.h.rearrange("(kc p) t -> p kc t", p=128)
    with ExitStack() as es:
        def sb(n, s, dt=F32):
            return es.enter_context(P.sbuf(n, s, dt))
        WS = sb("mws", [128, 4, 1024], F32)
        WPA = sb("wpa", [128, 4, 1024], BF16)
        WPB = sb("wpb", [128, 4, 1024], BF16)
        WO = sb("wo", [128, 8, 1024], BF16)
        IDB = sb("idb2", [128, 128], BF16)
        OAT = sb("oat", [128, 4, T], BF16)
        P.cp(IDB[:, :], c["IDF"][:, :])
        wpa = K.w_pa.h[l].rearrange("(kc p) n -> p kc n", p=128)
        wpb = K.w_pb.h[l].rearrange("(kc p) n -> p kc n", p=128)
        wo = K.w_out.h[l].rearrange("(kc p) n -> p kc n", p=128)
        P.dma("sp", WS[:, :, :], Opnd(("w_pa", l), wpa))
        P.cp(WPA[:, :, :], WS[:, :, :], e="pool")
        P.dma("sp", WS[:, :, :], Opnd(("w_pb", l), wpb))
        P.cp(WPB[:, :, :], WS[:, :, :], e="pool")
        for hh in range(2):
            P.dma("sp", WS[:, :, :], Opnd(("w_out", l), wo[:, hh * 4:(hh + 1) * 4, :]))
            P.cp(WO[:, hh * 4:(hh + 1) * 4, :], WS[:, :, :], e="pool")
        with ExitStack() as es2:
            def sb2(n, s, dt=F32):
                return es2.enter_context(P.sbuf(n, s, dt))
            O0 = [sb2("o0%d" % i, [128, 512]) for i in range(2)]
            O1 = [sb2("o1%d" % i, [128, 512]) for i in range(2)]
            ZT = [sb2("zt%d" % i, [128, 512]) for i in range(2)]
            SQ = [sb2("osq%d" % i, [128, 512]) for i in range(2)]
            SS = [sb2("oss%d" % i, [128, 8]) for i in range(2)]
            OA = [sb2("oa%d" % i, [128, 512], BF16) for i in range(2)]
            PT_ = [es2.enter_context(P.psum("mpt%d" % i, [128, 512], BF16)) for i in range(2)]
            S3 = [128, 8, 64]
            for ti in range(NTILE):
                u = ti % 2
                t0 = ti * 128
                o0, o1, zt, sq, ss, oa, pt = O0[u], O1[u], ZT[u], SQ[u], SS[u], OA[u], PT_[u]
                P.dma("sp", o0[:, :], Opnd("ODN", K.ODN.h[0, t0:t0 + 128, :]))
                P.dma("sp", o1[:, :], Opnd("ODN", K.ODN.h[1, t0:t0 + 128, :]))
                P.dma("sp", zt[:, :], Opnd("ZS", K.ZS.h[t0:t0 + 128, :]))
                P.tt(o0[:, :], o0[:, :], o1[:, :], ALU.add)
                P.act(sq[:, :], o0[:, :], AF.Square)
                P.red(ss[:, :], Opnd(sq.name, sq.h[:, :].rearrange("p (h e) -> p h e", h=8)), ALU.add)
                P.rsq(ss[:, :], ss[:, :], 1.0 / 64.0, EPS)
                o3 = Opnd(o0.name, o0.h[:, :].rearrange("p (h e) -> p h e", h=8))
                P.tt(o3, o3, bc(ss[:, :], 2, S3), ALU.mult)
                P.tt(o3, o3, bc(sm["DNW"][:, :], 1, S3), ALU.mult)
                P.tt(oa[:, :], o0[:, :], zt[:, :], ALU.mult)
                for kc in range(4):
                    P.tr(pt.k(kc)[:, kc * 128:(kc + 1) * 128], oa[:, kc * 128:(kc + 1) * 128], IDB[:, :])
                P.op("act", lambda g, a=OAT.h[:, :, t0:t0 + 128], b=pt.h[:, :].rearrange("p (k t) -> p k t", k=4): g.activation(a, b, AF.Copy),
                     [OAT.k(ti)[:, 0, 0:1]], [pt.k(kc)[:, 0:1] for kc in range(4)])
            P.barrier()
        with ExitStack() as es2:
            def sb2(n, s, dt=F32):
                return es2.enter_context(P.sbuf(n, s, dt))
            OB = [sb2("mob%d" % i, [128, 4, 512], BF16) for i in range(2)]
            XB = [sb2("mxb%d" % i, [128, 8, 512]) for i in range(2)]
            SGA = [sb2("sga%d" % i, [128, 512]) for i in range(2)]
            SGB = [sb2("sgb%d" % i, [128, 512]) for i in range(2)]
            TA = [sb2("mta%d" % i, [128, 512]) for i in range(2)]
            TB = [sb2("mtb%d" % i, [128, 512]) for i in range(2)]
            YT = [sb2("myt%d" % i, [128, 8, 512], BF16) for i in range(2)]
            PA = [es2.enter_context(P.psum("mpa%d" % i, [128, 512], F32)) for i in range(2)]
            PB = [es2.enter_context(P.psum("mpb%d" % i, [128, 512], F32)) for i in range(2)]
            PO = [es2.enter_context(P.psum("mpo%d" % i, [128, 512], F32)) for i in range(2)]
            it = 0
            for bi, (t0, n) in enumerate(BLOCKS):
                j = 1 if bi == 8 else 0
                ob, xb, yt = OB[bi % 2], XB[bi % 2], YT[bi % 2]
                P.dma("sp", ob[:, :, :n], Opnd("OBTd", OBv[:, :, t0:t0 + n]))
                P.dma("sp", xb[:, :, :n], Opnd(("XT", bi), XTv[:, :, t0:t0 + n]))
                for nn in range(8):
                    u = it % 2
                    it += 1
                    sga, sgb, ta, tb, pa, pb = SGA[u], SGB[u], TA[u], TB[u], PA[u], PB[u]
                    P.dma("sp", sga[:, :n], Opnd("SG", SGv[:, 0, nn, t0:t0 + n]))
                    P.dma("sp", sgb[:, :n], Opnd("SG", SGv[:, 1, nn, t0:t0 + n]))
                    for kc in range(4):
                        P.mm(pa[:, :n], WPA[:, kc, nn * 128:(nn + 1) * 128], Opnd(OAT.name, OAT.h[:, kc, t0:t0 + n]), start=(kc == 0), stop=(kc == 3))
                    for kc in range(4):
                        P.mm(pb[:, :n], WPB[:, kc, nn * 128:(nn + 1) * 128], ob[:, kc, :n], start=(kc == 0), stop=(kc == 3))
                    P.tt(ta[:, :n], pa[:, :n], sga[:, :n], ALU.mult)
                    P.tt(tb[:, :n], pb[:, :n], sgb[:, :n], ALU.mult)
                    P.tt(yt.k(nn)[:, nn, :n], ta[:, :n], tb[:, :n], ALU.add, e="pool")
                for m in range(8):
                    po = PO[m % 2]
                    for nn in range(8):
                        P.mm(po[:, :n], WO[:, nn, m * 128:(m + 1) * 128], yt.k(nn)[:, nn, :n], start=(nn == 0), stop=(nn == 7))
                    g1 = modv(K, 2, m, j)
                    P.op("dve", lambda g, a=xb.h[:, m, :n], b=po.h[:, :n], s_=g1.ap: g.scalar_tensor_tensor(a, b, s_, a, ALU.mult, ALU.add),
                         [xb.k(m)[:, m, 0:1]], [po[:, :n], g1, xb[:, 0, 0:1]])
                P.dma("sp", Opnd(("XT", bi), XTv[:, :, t0:t0 + n]), Opnd(xb.name, xb.h[:, :, :n]),
                      extra_reads=[xb.k(m)[:, m, 0:1] for m in range(8)])
            P.barrier()


def stage_peer(P, K, l, tiles=None):
    c = K.c
    sm = K.small
    XTv = K.XT.h.rearrange("(fc p) t -> p fc t", p=128)
    wq = K.peer_wq.h[l].rearrange("(kc p) n -> p kc n", p=128)
    Utab = Opnd(("peer_u", l), getattr(K, "peer_u%d" % l).h[:, :])
    Vtab = Opnd(("peer_v", l), getattr(K, "peer_v%d" % l).h[:, :])
    NB = 8
    with ExitStack() as es:
        def sb(n, s, dt=F32):
            return es.enter_context(P.sbuf(n, s, dt))
        WQ = sb("wq", [128, 8, 2048], BF16)
        with P.sbuf("pws", [128, 8, 512], F32) as WS:
            for q4 in range(4):
                P.dma("sp", WS[:, :, :], Opnd(("peer_wq", l), wq[:, :, q4 * 512:(q4 + 1) * 512]))
                P.cp(WQ[:, :, q4 * 512:(q4 + 1) * 512], WS[:, :, :], e="pool")
            P.barrier()
        IDB = sb("idb3", [128, 128], BF16)
        P.cp(IDB[:, :], c["IDF"][:, :])
        XB = [sb("pxb%d" % i, [128, 8, 128]) for i in range(2)]
        SQ = sb("psq", [128, 8, 128])
        RS = sb("prs", [128, 128])
        TMP = [sb("ptmp%d" % i, [128, 128]) for i in range(2)]
        HT = sb("pht", [128, 8, 128], BF16)
        QTs = sb("qts", [128, 16, 128])
        SCR = sb("scr", [128, 16, 128])
        SCR2 = sb("scr2", [128, 16, 128])
        TS = sb("tsv", [128, 16, 16])
        TIu = sb("tiu", [128, 16, 16], U32)
        TIf = sb("tif", [128, 16, 16])
        CS = sb("cs", [128, 8, 256])
        CS2 = sb("cs2", [128, 8, 256])
        BS = sb("bs", [128, 8, 16])
        POSu = sb("posu", [128, 8, 16], U32)
        Au = sb("au", [128, 8, 16], U32)
        Bu = sb("bu", [128, 8, 16], U32)
        Af = sb("af", [128, 8, 16])
        Bf = sb("bf", [128, 8, 16])
        EQ = sb("eq", [128, 8, 16, 16])
        ISel = sb("isel", [128, 8, 16])
        JSel = sb("jsel", [128, 8, 16])
        IDXf = sb("idxf", [128, 128])
        IDX = sb("idx", [128, 128], I32)
        DD = sb("dd", [128, 8, 16])
        SMs = sb("sms", [128, 8])
        GATE = sb("gate", [128, 8, 16])
        HTOK = sb("htok", [128, 1024])
        UG = [sb("ug%d" % i, [128, 1024]) for i in range(NB)]
        JUNK = [sb("junk%d" % i, [128, 1024], BF16) for i in range(4)]
        APRE = sb("apre", [128, 128])
        G1 = sb("g1", [128, 128])
        G2 = sb("g2", [128, 128])
        COEF = sb("coef", [128, 128])
        DIAG = [sb("diag%d" % i, [128, 128]) for i in range(4)]
        FTOK = sb("ftok", [128, 1024])
        PQ = es.enter_context(P.psum("ppq", [128, 128], F32))
        PACC = es.enter_context(P.psum("pacc", [128, 1024], F32))
        PSC = es.enter_context(P.psum("psc", [128, 8, 128], F32))
        PH = es.enter_context(P.psum("pph", [128, 1024], BF16))
        PF = es.enter_context(P.psum("ppf", [128, 8, 128], F32))
        S4 = [128, 8, 16, 16]
        for ti in (tiles if tiles is not None else range(NTILE)):
            t0 = ti * 128
            j = 1 if t0 >= LT else 0
            xb = XB[ti % 2]
            P.dma("sp", xb[:, :, :], Opnd(("XT", min(t0 // 512, 8)), XTv[:, :, t0:t0 + 128]))
            P.act(SQ[:, :, :], xb[:, :, :], AF.Square)
            for fc in range(8):
                P.mm(PSC[:, 0, :], c["ONESM"][:, :], SQ[:, fc, :], start=(fc == 0), stop=(fc == 7))
            P.rsq(RS[:, :], PSC[:, 0, :], 1.0, EPS)
            for fc in range(8):
                tmp = TMP[fc % 2]
                P.stt(tmp[:, :], xb[:, fc, :], K.A2[:, fc, j:j + 1], RS[:, :], ALU.mult, ALU.mult)
                P.act(HT.k(fc)[:, fc, :], tmp[:, :], AF.Identity, bias=modv(K, 3, fc, j))
            for hp in range(16):
                for kc in range(8):
                    P.mm(PQ[:, :], WQ[:, kc, hp * 128:(hp + 1) * 128], HT.k(kc)[:, kc, :], start=(kc == 0), stop=(kc == 7))
                if hp % 2 == 0:
                    P.act(QTs.k(hp)[:, hp, :], PQ[:, :], AF.Copy)
                else:
                    P.cp(QTs.k(hp)[:, hp, :], PQ[:, :])
            for half in range(2):
                for h8 in range(8):
                    hp = half * 8 + h8
                    P.mm(PSC[:, h8, :], QTs.k(hp)[:, hp, :], sm["KEYST"][:, hp, :])
                P.act(SCR.k(half)[:, half * 8:(half + 1) * 8, :], PSC[:, :, :], AF.Copy)
            for hp in range(16):
                P.op("dve", lambda g, hp=hp: g.max(TS.h[:, hp, 0:8], SCR.h[:, hp, :]), [TS.k(hp)[:, hp, 0:8]], [SCR.k(hp // 8)[:, hp, :]])
                P.op("dve", lambda g, hp=hp: g.max_index(TIu.h[:, hp, 0:8], TS.h[:, hp, 0:8], SCR.h[:, hp, :]), [TIu.k(hp)[:, hp, 0:8]], [TS.k(hp)[:, hp, 0:8], SCR.k(hp // 8)[:, hp, :]])
                P.op("dve", lambda g, hp=hp: g.match_replace(SCR2.h[:, hp, :], TS.h[:, hp, 0:8], SCR.h[:, hp, :], -1e30), [SCR2.k(hp)[:, hp, :]], [TS.k(hp)[:, hp, 0:8], SCR.k(hp // 8)[:, hp, :]])
                P.op("dve", lambda g, hp=hp: g.max(TS.h[:, hp, 8:16], SCR2.h[:, hp, :]), [TS.k((hp, 1))[:, hp, 8:16]], [SCR2.k(hp)[:, hp, :]])
                P.op("dve", lambda g, hp=hp: g.max_index(TIu.h[:, hp, 8:16], TS.h[:, hp, 8:16], SCR2.h[:, hp, :]), [TIu.k((hp, 1))[:, hp, 8:16]], [TS.k((hp, 1))[:, hp, 8:16], SCR2.k(hp)[:, hp, :]])
            ts_all = [TS.k(hp)[:, hp, 0:8] for hp in range(16)] + [TS.k((hp, 1))[:, hp, 8:16] for hp in range(16)]
            ti_all = [TIu.k(hp)[:, hp, 0:8] for hp in range(16)] + [TIu.k((hp, 1))[:, hp, 8:16] for hp in range(16)]
            P.op("dve", lambda g: g.tensor_copy(TIf.h[:, :, :], TIu.h[:, :, :]), [TIf[:, :, :]], ti_all)
            ts4 = TS.h[:, :, :].rearrange("p (h two) k -> p h two k", two=2)
            ti4 = TIf.h[:, :, :].rearrange("p (h two) k -> p h two k", two=2)
            cs4 = CS.h[:, :, :].rearrange("p h (a b) -> p h a b", a=16)
            P.op("dve", lambda g: g.tensor_tensor(cs4, ts4[:, :, 0, :].unsqueeze(3).to_broadcast(S4), ts4[:, :, 1, :].unsqueeze(2).to_broadcast(S4), ALU.add),
                 [CS[:, :, :]], ts_all)
            for h in range(8):
                P.op("dve", lambda g, h=h: g.max(BS.h[:, h, 0:8], CS.h[:, h, :]), [BS.k(h)[:, h, 0:8]], [CS[:, h, :]])
                P.op("dve", lambda g, h=h: g.max_index(POSu.h[:, h, 0:8], BS.h[:, h, 0:8], CS.h[:, h, :]), [POSu.k(h)[:, h, 0:8]], [BS.k(h)[:, h, 0:8], CS[:, h, :]])
                P.op("dve", lambda g, h=h: g.match_replace(CS2.h[:, h, :], BS.h[:, h, 0:8], CS.h[:, h, :], -1e30), [CS2.k(h)[:, h, :]], [BS.k(h)[:, h, 0:8], CS[:, h, :]])
                P.op("dve", lambda g, h=h: g.max(BS.h[:, h, 8:16], CS2.h[:, h, :]), [BS.k((h, 1))[:, h, 8:16]], [CS2.k(h)[:, h, :]])
                P.op("dve", lambda g, h=h: g.max_index(POSu.h[:, h, 8:16], BS.h[:, h, 8:16], CS2.h[:, h, :]), [POSu.k((h, 1))[:, h, 8:16]], [BS.k((h, 1))[:, h, 8:16], CS2.k(h)[:, h, :]])
            bs_all = [BS.k(h)[:, h, 0:8] for h in range(8)] + [BS.k((h, 1))[:, h, 8:16] for h in range(8)]
            pos_all = [POSu.k(h)[:, h, 0:8] for h in range(8)] + [POSu.k((h, 1))[:, h, 8:16] for h in range(8)]
            P.op("dve", lambda g: g.tensor_scalar(Au.h[:, :, :], POSu.h[:, :, :], 4, None, ALU.logical_shift_right), [Au[:, :, :]], pos_all)
            P.op("dve", lambda g: g.tensor_scalar(Bu.h[:, :, :], POSu.h[:, :, :], 15, None, ALU.bitwise_and), [Bu[:, :, :]], pos_all)
            P.cp(Af[:, :, :], Au[:, :, :])
            P.cp(Bf[:, :, :], Bu[:, :, :])
            io4 = c["IOTA16"].h[:, :].unsqueeze(1).unsqueeze(1).to_broadcast(S4)
            for (sel, xf, which) in ((ISel, Af, 0), (JSel, Bf, 1)):
                P.op("dve", lambda g, xf=xf: g.tensor_tensor(EQ.h[:, :, :, :], io4, xf.h[:, :, :].unsqueeze(3).to_broadcast(S4), ALU.is_equal),
                     [EQ[:, :, :, :]], [xf[:, :, :], c["IOTA16"][:, :]])
                P.op("dve", lambda g, which=which: g.tensor_tensor(EQ.h[:, :, :, :], EQ.h[:, :, :, :], ti4[:, :, which, :].unsqueeze(2).to_broadcast(S4), ALU.mult),
                     [EQ[:, :, :, :]], [EQ[:, :, :, :], TIf[:, :, :]])
                P.red(sel[:, :, :], EQ[:, :, :, :], ALU.add)
            idx3 = IDXf.h[:, :].rearrange("p (h k) -> p h k", h=8)
            P.op("dve", lambda g: g.scalar_tensor_tensor(idx3, ISel.h[:, :, :], 128.0, JSel.h[:, :, :], ALU.mult, ALU.add),
                 [IDXf[:, :]], [ISel[:, :, :], JSel[:, :, :]])
            P.cp(IDX[:, :], IDXf[:, :])
            P.op("dve", lambda g: g.tensor_tensor(DD.h[:, :, :], BS.h[:, :, :], BS.h[:, :, 0:1].to_broadcast([128, 8, 16]), ALU.subtract),
                 [DD[:, :, :]], bs_all)
            P.act(DD[:, :, :], DD[:, :, :], AF.Exp)
            P.red(SMs[:, :], DD[:, :, :], ALU.add)
            P.op("dve", lambda g: g.reciprocal(SMs.h[:, :], SMs.h[:, :]), [SMs[:, :]], [SMs[:, :]])
            P.tt(GATE[:, :, :], DD[:, :, :], bc(SMs[:, :], 2, [128, 8, 16]), ALU.mult)
            for fc in range(8):
                P.tr(PH.k(fc)[:, fc * 128:(fc + 1) * 128], HT.k(fc)[:, fc, :], IDB[:, :])
            P.op("act", lambda g: g.activation(HTOK.h[:, :], PH.h[:, :], AF.Copy), [HTOK[:, :]], [PH.k(fc)[:, 0:1] for fc in range(8)])
            for k in range(128):
                buf = UG[k % NB]
                P.gather(buf[:, :], Utab, IDX[:, k:k + 1], 16384)
                jk = JUNK[k % 4]
                P.op("dve", lambda g, k=k, buf=buf, jk=jk: g.scalar_tensor_tensor(jk.h[:, :], HTOK.h[:, :], 1.0, buf.h[:, :], ALU.mult, ALU.mult, accum_out=APRE.h[:, k:k + 1]),
                     [jk[:, :], APRE.k(k)[:, k:k + 1]], [HTOK[:, :], buf[:, :]])
            apre_all = [APRE.k(k)[:, k:k + 1] for k in range(128)]
            P.op("dve", lambda g: g.tensor_tensor(G1.h[:, :], APRE.h[:, :], APRE.h[:, :], ALU.mult), [G1[:, :]], apre_all)
            P.op("dve", lambda g: g.tensor_tensor(G1.h[:, :], G1.h[:, :], APRE.h[:, :], ALU.mult), [G1[:, :]], apre_all + [G1[:, :]])
            P.op("dve", lambda g: g.scalar_tensor_tensor(G2.h[:, :], G1.h[:, :], 0.044715, APRE.h[:, :], ALU.mult, ALU.add), [G2[:, :]], apre_all + [G1[:, :]])
            P.act(G2[:, :], G2[:, :], AF.Sigmoid, scale=1.5957691216057308)
            P.op("dve", lambda g: g.tensor_tensor(G1.h[:, :], G2.h[:, :], APRE.h[:, :], ALU.mult), [G1[:, :]], apre_all + [G2[:, :]])
            P.tt(COEF[:, :], G1[:, :], Opnd(GATE.name, GATE.h[:, :, :].rearrange("p h k -> p (h k)")), ALU.mult)
            for k in range(128):
                buf = UG[k % NB]
                dg = DIAG[k % 4]
                P.gather(buf[:, :], Vtab, IDX[:, k:k + 1], 16384)
                P.ts(dg[:, :], c["IDF"][:, :], COEF[:, k:k + 1], ALU.mult)
                P.mm(PACC[:, 0:512], dg[:, :], buf[:, 0:512], start=(k == 0), stop=(k == 127))
                P.mm(PACC[:, 512:1024], dg[:, :], buf[:, 512:1024], start=(k == 0), stop=(k == 127))
            P.act(FTOK[:, :], PACC[:, :], AF.Copy)
            for fc in range(8):
                P.tr(PF.k(fc)[:, fc, :], FTOK[:, fc * 128:(fc + 1) * 128], c["IDF"][:, :])
            for fc in range(8):
                g2 = modv(K, 5, fc, j)
                P.op("dve", lambda g, fc=fc, g2=g2, xb=xb: g.scalar_tensor_tensor(xb.h[:, fc, :], PF.h[:, fc, :], g2.ap, xb.h[:, fc, :], ALU.mult, ALU.add),
                     [xb.k(fc)[:, fc, 0:1]], [PF.k(f_)[:, f_, :] for f_ in range(8)] + [g2, xb[:, 0, 0:1]])
            P.dma("sp", Opnd(("XT", min(t0 // 512, 8)), XTv[:, :, t0:t0 + 128]), Opnd(xb.name, xb.h[:, :, :]),
                  extra_reads=[xb.k(fc)[:, fc, 0:1] for fc in range(8)])
        P.barrier()
```

```python
import numpy as np
from contextlib import ExitStack, contextmanager
import concourse.bass as bass
import concourse.mybir as mybir
from concourse.bass_utils import run_bass_kernel_spmd

F32 = mybir.dt.float32
BF16 = mybir.dt.bfloat16
I32 = mybir.dt.int32
U32 = mybir.dt.uint32
AF = mybir.ActivationFunctionType
ALU = mybir.AluOpType
AX = mybir.AxisListType

D = 1024
LT = 4096
CT = 256
T = LT + CT
DEPTH = 4
IN_W = 5664
EPS = 1e-6
BLOCKS = [(i * 512, 512) for i in range(8)] + [(LT, CT)]
NTILE = T // 128


class Opnd:
    __slots__ = ("key", "ap")

    def __init__(self, key, ap):
        self.key = key
        self.ap = ap


class _Sub:
    def __init__(self, t, sub):
        self.t = t
        self.sub = sub

    def __getitem__(self, idx):
        return Opnd((self.t.name, self.sub), self.t.h[idx])


class Tile:
    def __init__(self, h, name):
        self.h = h
        self.name = name

    def __getitem__(self, idx):
        return Opnd(self.name, self.h[idx])

    def k(self, sub):
        return _Sub(self, sub)


def W(o, ap):
    return Opnd(o.key, ap)


class Prog:
    def __init__(self, nc, es):
        self.nc = nc
        self.es = es
        self.engs = {"pe": nc.tensor, "dve": nc.vector, "act": nc.scalar, "pool": nc.gpsimd, "sp": nc.sync}
        self.sem = {}
        self.cnt = {}
        for e in ("pe", "dve", "act", "pool"):
            self.sem[e] = es.enter_context(nc.semaphore("c_" + e))
            self.cnt[e] = 0
        self.dq = {}
        self.dptr = {}
        for q, n in (("sp", 32), ("pool", 24), ("act", 4)):
            ks = []
            for i in range(n):
                k = "d_%s%d" % (q, i)
                self.sem[k] = es.enter_context(nc.semaphore(k))
                self.cnt[k] = 0
                ks.append(k)
            self.dq[q] = ks
            self.dptr[q] = 0
        self.seen = {e: {} for e in self.engs}
        self.lw = {}
        self.rd = {}
        self.uid = 0
        self.ninst = 0
        self.psum_names = set()

    @contextmanager
    def sbuf(self, name, shape, dt):
        self.uid += 1
        nm = "%s_%d" % (name, self.uid)
        with self.nc.sbuf_tensor(nm, list(shape), dt) as h:
            yield Tile(h, nm)

    @contextmanager
    def psum(self, name, shape, dt):
        self.uid += 1
        nm = "%s_%d" % (name, self.uid)
        esz = 2 if dt == BF16 else 4
        n = 1
        for d_ in shape[1:]:
            n *= d_
        per_bank = 2048 // esz
        npad = ((n + per_bank - 1) // per_bank) * per_bank
        self.psum_names.add(nm)
        with self.nc.psum_tensor(nm, [shape[0], npad], dt) as h:
            v = h[:, 0:n]
            if len(shape) == 3:
                v = v.rearrange("p (a b) -> p a b", a=shape[1])
            elif len(shape) == 4:
                v = v.rearrange("p (a b c) -> p a b c", a=shape[1], b=shape[2])
            yield Tile(v, nm)

    def _deps(self, reads, writes):
        d = {}

        def add(tok):
            if tok is None:
                return
            k, v = tok
            if d.get(k, 0) < v:
                d[k] = v

        for r in reads:
            add(self.lw.get(r))
        for w in writes:
            add(self.lw.get(w))
            for k, v in self.rd.get(w, {}).items():
                add((k, v))
        return d

    def _wait(self, e, deps):
        eng = self.engs[e]
        seen = self.seen[e]
        for k, v in deps.items():
            if e == "pe" and k == "pe":
                continue
            if seen.get(k, 0) < v:
                eng.wait_ge(self.sem[k], v)
                seen[k] = v

    def _commit(self, tok, reads, writes):
        for w in writes:
            self.lw[w] = tok
            self.rd[w] = {}
        k, v = tok
        for r in reads:
            m = self.rd.setdefault(r, {})
            if m.get(k, 0) < v:
                m[k] = v

    def _is_psum(self, key):
        return (key if isinstance(key, str) else key[0]) in self.psum_names

    def op(self, e, fn, outs, ins):
        reads = [o.key for o in ins]
        writes = [o.key for o in outs]
        writes = writes + [k for k in reads if self._is_psum(k)]
        self._wait(e, self._deps(reads, writes))
        ins_ = fn(self.engs[e])
        self.cnt[e] += 1
        ins_.then_inc(self.sem[e], 1)
        self._commit((e, self.cnt[e]), reads, writes)
        self.ninst += 1

    def dma(self, q, out, in_, extra_reads=(), **kw):
        reads = [in_.key] + [o.key for o in extra_reads]
        writes = [out.key]
        ks = self.dq[q]
        k = ks[self.dptr[q] % len(ks)]
        self.dptr[q] += 1
        deps = self._deps(reads, writes)
        deps[k] = self.cnt[k]
        self._wait(q, deps)
        ins_ = self.engs[q].dma_start(out=out.ap, in_=in_.ap, **kw)
        self.cnt[k] += 16
        ins_.then_inc(self.sem[k], 16)
        self._commit((k, self.cnt[k]), reads, writes)
        self.ninst += 1

    def gather(self, out, table, idx, nrows):
        q = "pool"
        reads = [table.key, idx.key]
        writes = [out.key]
        ks = self.dq[q]
        k = ks[self.dptr[q] % len(ks)]
        self.dptr[q] += 1
        deps = self._deps(reads, writes)
        deps[k] = self.cnt[k]
        self._wait(q, deps)
        ins_ = self.nc.gpsimd.indirect_dma_start(
            out=out.ap, out_offset=None, in_=table.ap,
            in_offset=bass.IndirectOffsetOnAxis(ap=idx.ap, axis=0))
        self.cnt[k] += 16
        ins_.then_inc(self.sem[k], 16)
        self._commit((k, self.cnt[k]), reads, writes)
        self.ninst += 1

    def barrier(self):
        for e in self.engs:
            self._wait(e, dict(self.cnt))

    def mm(self, out, lhsT, rhs, start=True, stop=True):
        self.op("pe", lambda g: g.matmul(out.ap, lhsT.ap, rhs.ap, start=start, stop=stop), [out], [lhsT, rhs])

    def tr(self, out, in_, ident):
        self.op("pe", lambda g: g.transpose(out.ap, in_.ap, ident.ap), [out], [in_, ident])

    def act(self, out, in_, func, bias=None, scale=None, e="act", accum=None):
        kw = {}
        ins = [in_]
        if bias is not None:
            if isinstance(bias, Opnd):
                kw["bias"] = bias.ap
                ins.append(bias)
            else:
                kw["bias"] = bias
        if scale is not None:
            if isinstance(scale, Opnd):
                kw["scale"] = scale.ap
                ins.append(scale)
            else:
                kw["scale"] = scale
        outs = [out]
        if accum is not None:
            kw["accum_out"] = accum.ap
            outs.append(accum)
        self.op("act", lambda g: g.activation(out.ap, in_.ap, func, **kw), outs, ins)

    def tt(self, out, in0, in1, op, e="dve"):
        self.op(e, lambda g: g.tensor_tensor(out.ap, in0.ap, in1.ap, op), [out], [in0, in1])

    def ts(self, out, in0, s1, op0, s2=None, op1=None, e="dve"):
        ins = [in0]
        a1 = s1
        if isinstance(s1, Opnd):
            a1 = s1.ap
            ins.append(s1)
        a2 = s2
        if isinstance(s2, Opnd):
            a2 = s2.ap
            ins.append(s2)
        if op1 is None:
            self.op(e, lambda g: g.tensor_scalar(out.ap, in0.ap, a1, None, op0), [out], ins)
        else:
            self.op(e, lambda g: g.tensor_scalar(out.ap, in0.ap, a1, a2, op0, op1), [out], ins)

    def stt(self, out, in0, scalar, in1, op0, op1, e="dve"):
        ins = [in0, in1]
        a = scalar
        if isinstance(scalar, Opnd):
            a = scalar.ap
            ins.append(scalar)
        self.op(e, lambda g: g.scalar_tensor_tensor(out.ap, in0.ap, a, in1.ap, op0, op1), [out], ins)

    def rsq(self, out, in_, mult, add):
        self.act(out, in_, AF.Ln, bias=add, scale=mult)
        self.act(out, out, AF.Exp, scale=-0.5)

    def ttr(self, out, in0, in1, accum, op0=ALU.mult, op1=ALU.add):
        self.op("dve", lambda g: g.tensor_tensor_reduce(out.ap, in0.ap, in1.ap, 1.0, 0.0, op0, op1, accum.ap), [out, accum], [in0, in1])

    def cp(self, out, in_, e="dve"):
        self.op(e, lambda g: g.tensor_copy(out.ap, in_.ap), [out], [in_])

    def red(self, out, in_, op, axis=AX.X, e="dve"):
        self.op(e, lambda g: g.tensor_reduce(out.ap, in_.ap, axis, op), [out], [in_])

    def memset(self, out, val, e="dve"):
        self.op(e, lambda g: g.memset(out.ap, val), [out], [])


def _consts():
    c = {}
    c["IDF"] = np.eye(128, dtype=np.float32)
    c["ONESM"] = np.full((128, 128), 1.0 / 1024.0, np.float32)
    blk = np.zeros((128, 128), np.float32)
    blk[:64, :64] = 1
    blk[64:, 64:] = 1
    c["BLK"] = blk
    tri = np.zeros((128, 128), np.float32)
    i = np.arange(64)
    tri[:64, :64] = (i[:, None] <= i[None, :])
    tri[64:, 64:] = (i[:, None] >= i[None, :])
    c["TRI"] = tri
    c["STRI"] = tri - np.eye(128, dtype=np.float32)
    ind = np.zeros((128, 2), np.float32)
    ind[:64, 0] = 1
    ind[64:, 1] = 1
    c["IND"] = ind
    rot = np.zeros((128, 128), np.float32)
    for m in range(128):
        if m % 64 < 32:
            rot[m + 32, m] = -1.0
        else:
            rot[m - 32, m] = 1.0
    c["ROT"] = rot
    t = np.arange(LT)
    row = (t // 64).astype(np.float32)
    col = (t % 64).astype(np.float32)
    inv = (np.float32(10000.0) ** (-np.arange(16, dtype=np.float32) / np.float32(16))).astype(np.float32)
    ang = np.concatenate([row[:, None] * inv, col[:, None] * inv], axis=-1).astype(np.float32)
    cs = np.cos(ang).astype(np.float32).T
    sn = np.sin(ang).astype(np.float32).T
    c["COS"] = np.ascontiguousarray(np.tile(cs, (4, 1)))
    c["SIN"] = np.ascontiguousarray(np.tile(sn, (4, 1)))
    c["IOTA16"] = np.tile(np.arange(16, dtype=np.float32)[None, :], (128, 1))
    return c


def _na_bias_tables(rpb):
    H = rpb.shape[0]
    kc = np.arange(64)[:, None]
    qc = np.arange(64)[None, :]
    ws = np.clip(qc - 8, 0, 48)
    inwin = (kc >= ws) & (kc < ws + 16)
    off = np.clip(kc - qc + 15, 0, 30)
    out = np.full((2, 64, H, 16, 64), -30000.0, np.float32)
    combos = [(dr0, 1, 1) for dr0 in range(-7, 7)] + [(-5, 0, 1), (3, 1, 0)]
    for di, (dr0, v0, v1) in enumerate(combos):
        for a in range(2):
            dr = dr0 + a
            if dr < -7 or dr > 7 or not (v0, v1)[a]:
                continue
            g = rpb[:, dr + 7, :][:, off]
            g = np.where(inwin[None], g, np.float32(-30000.0))
            out[a, :, :, di, :] = g.transpose(1, 0, 2)
    return np.ascontiguousarray(out.reshape(128, H, 16, 64))


def na_sched(r):
    rs = min(max(r - 4, 0), 56)
    out = []
    for kr0 in range(rs - (rs % 2), rs + 8, 2):
        v0 = rs <= kr0 < rs + 8
        v1 = rs <= kr0 + 1 < rs + 8
        dr0 = kr0 - r
        if v0 and v1:
            tbl = dr0 + 7
        elif v1:
            assert dr0 == -5
            tbl = 14
        else:
            assert dr0 == 3 and v0
            tbl = 15
        out.append((kr0 // 2, tbl))
    return out


def _layer_small(inp, l):
    d = {}
    d["ADABT"] = np.ascontiguousarray(np.repeat(inp["ada_b"][l].reshape(48, 128).T[:, :, None], 2, axis=2))
    d["N1W"] = np.ascontiguousarray(np.repeat(inp["norm1_w"][l].reshape(8, 128).T[:, :, None], 2, axis=2))
    d["N2W"] = np.ascontiguousarray(np.repeat(inp["norm2_w"][l].reshape(8, 128).T[:, :, None], 2, axis=2))
    d["CONVW"] = np.ascontiguousarray(inp["dn_conv_w"][l].T.reshape(12, 128, 5).transpose(1, 0, 2))
    d["ALOG"] = np.ascontiguousarray(np.tile(inp["dn_a_log"][l].reshape(1, 16), (128, 1)))
    d["DTB"] = np.ascontiguousarray(np.tile(inp["dn_dt_bias"][l].reshape(1, 16), (128, 1)))
    d["DNW"] = np.ascontiguousarray(np.tile(inp["dn_norm_w"][l].reshape(1, 64), (128, 1)))
    qk = np.stack([np.tile(inp["na_qnorm_w"][l], 2), np.tile(inp["na_knorm_w"][l], 2)], axis=1)
    d["NAW"] = np.ascontiguousarray(qk)
    d["BIAST"] = _na_bias_tables(inp["na_rpb"][l])
    d["KEYST"] = np.ascontiguousarray(inp["peer_keys"][l].transpose(3, 0, 1, 2).reshape(128, 16, 128))
    return d


SMALL_SHAPES = {"ADABT": (128, 48, 2), "N1W": (128, 8, 2), "N2W": (128, 8, 2), "CONVW": (128, 12, 5), "ALOG": (128, 16),
                "DTB": (128, 16), "DNW": (128, 64), "NAW": (128, 2), "KEYST": (128, 16, 128)}
BIAST_SHAPE = (128, 8, 16, 64)
CONST_SHAPES = {"IDF": (128, 128), "ONESM": (128, 128), "BLK": (128, 128), "TRI": (128, 128), "STRI": (128, 128),
                "IND": (128, 2), "ROT": (128, 128), "COS": (128, LT), "SIN": (128, LT), "IOTA16": (128, 16)}
BIG_SHAPES = {"ada_w": (DEPTH, D, 6 * D), "w_in": (DEPTH, D, IN_W), "w_pa": (DEPTH, 512, D), "w_pb": (DEPTH, 512, D),
              "w_out": (DEPTH, D, D), "peer_wq": (DEPTH, D, 2048)}
for _l in range(DEPTH):
    BIG_SHAPES["peer_u%d" % _l] = (16384, D)
    BIG_SHAPES["peer_v%d" % _l] = (16384, D)


class Ctx:
    pass


def dram_in(nc, K, name, shape, dt=F32):
    h = nc.dram_tensor(name, list(shape), dt, kind="ExternalInput")
    t = Tile(h.ap(), name)
    setattr(K, name, t)
    return t


def dram_scratch(nc, K, name, shape, dt=F32, kind="Internal"):
    h = nc.dram_tensor(name, list(shape), dt, kind=kind)
    t = Tile(h.ap(), name)
    setattr(K, name, t)
    return t


def stage_mod(P, K, l):
    with ExitStack() as es:
        Wb = [es.enter_context(P.sbuf("adaw%d" % i, [128, 8, 512], F32)) for i in range(2)]
        ps = es.enter_context(P.psum("modps", [128, 96], F32))
        aw = K.ada_w.h[l].rearrange("(kc p) n -> p kc n", p=128)
        for blk in range(12):
            Wt = Wb[blk % 2]
            P.dma("sp", Wt[:, :, :], Opnd(("ada_w", l), aw[:, :, blk * 512:(blk + 1) * 512]))
            for n4 in range(4):
                n = blk * 4 + n4
                for kc in range(8):
                    P.mm(ps[:, 2 * n:2 * n + 2], Wt[:, kc, n4 * 128:(n4 + 1) * 128], K.SC[:, kc, :],
                         start=(kc == 0), stop=(kc == 7))
        sm = K.small
        for n in range(48):
            P.tt(K.MOD.k(n)[:, n, :], ps[:, 2 * n:2 * n + 2], sm["ADABT"][:, n, :], ALU.add)
        for fc in range(8):
            P.stt(K.A1[:, fc, :], K.MOD.k(8 + fc)[:, 8 + fc, :], 1.0, sm["N1W"][:, fc, :], ALU.add, ALU.mult)
            P.stt(K.A2[:, fc, :], K.MOD.k(32 + fc)[:, 32 + fc, :], 1.0, sm["N2W"][:, fc, :], ALU.add, ALU.mult)
        P.barrier()


def modv(K, seg, fc, j):
    n = seg * 8 + fc
    return K.MOD.k(n)[:, n, j:j + 1]


def stage_norm(P, K, A, seg_shift, hT):
    XTv = K.XT.h.rearrange("(fc p) t -> p fc t", p=128)
    with ExitStack() as es:
        XB = [es.enter_context(P.sbuf("xb%d" % i, [128, 8, 512], F32)) for i in range(2)]
        SQ = es.enter_context(P.sbuf("sq", [128, 8, 512], F32))
        RS = es.enter_context(P.sbuf("rstd", [128, 512], F32))
        TMP = [es.enter_context(P.sbuf("ntmp%d" % i, [128, 512], F32)) for i in range(2)]
        ps = es.enter_context(P.psum("nps", [128, 512], F32))
        for bi, (t0, n) in enumerate(BLOCKS):
            j = 1 if bi == 8 else 0
            xb = XB[bi % 2]
            P.dma("sp", xb[:, :, :n], Opnd(("XT", bi), XTv[:, :, t0:t0 + n]))
            P.act(SQ[:, :, :n], xb[:, :, :n], AF.Square)
            for fc in range(8):
                P.mm(ps[:, :n], K.c["ONESM"][:, :], SQ[:, fc, :n], start=(fc == 0), stop=(fc == 7))
            P.rsq(RS[:, :n], ps[:, :n], 1.0, EPS)
            for fc in range(8):
                tmp = TMP[fc % 2]
                P.stt(tmp[:, :n], xb[:, fc, :n], A[:, fc, j:j + 1], RS[:, :n], ALU.mult, ALU.mult)
                P.act(hT.k(bi)[:, fc, t0:t0 + n], tmp[:, :n], AF.Identity, bias=modv(K, seg_shift, fc, j))
        P.barrier()


C_DQKV, C_Z, C_AB, C_NQ, C_NK, C_NV, C_GA, C_GB = 0, 1536, 2048, 2080, 2592, 3104, 3616, 4640
RAWLEN = LT + CT + 8
RAW_L0 = 2
RAW_C0 = LT + 6


def load_w_bf16(P, K, wsrc_key, wview, WS, WB, ncols):
    nk = WB.h.shape[1]
    P.dma("sp", WS[:, :nk, :ncols], Opnd(wsrc_key, wview))
    P.cp(WB[:, :, :ncols], WS[:, :nk, :ncols], e="pool")


def stage_inproj(P, K, l, hT):
    win = K.w_in.h[l].rearrange("(kc p) n -> p kc n", p=128)
    sm = K.small
    c = K.c
    with ExitStack() as es:
        WS = es.enter_context(P.sbuf("ws_t", [128, 8, 512], F32))
        WZ = es.enter_context(P.sbuf("wz", [128, 8, 512], BF16))
        WV = es.enter_context(P.sbuf("wv", [128, 8, 512], BF16))
        WA = es.enter_context(P.sbuf("wa", [128, 8, 32], BF16))
        NEGA = es.enter_context(P.sbuf("nega", [128, 16], F32))
        load_w_bf16(P, K, ("w_in", l), win[:, :, C_Z:C_Z + 512], WS, WZ, 512)
        load_w_bf16(P, K, ("w_in", l), win[:, :, C_NV:C_NV + 512], WS, WV, 512)
        load_w_bf16(P, K, ("w_in", l), win[:, :, C_AB:C_AB + 32], WS, WA, 32)
        P.act(NEGA[:, :], sm["ALOG"][:, :], AF.Exp)
        P.ts(NEGA[:, :], NEGA[:, :], -1.0, ALU.mult)
        PZ = [es.enter_context(P.psum("pz%d" % i, [128, 512], F32)) for i in range(2)]
        PV = [es.enter_context(P.psum("pv%d" % i, [128, 512], F32)) for i in range(2)]
        PA = [es.enter_context(P.psum("pa%d" % i, [128, 32], F32)) for i in range(2)]
        ZO = [es.enter_context(P.sbuf("zo%d" % i, [128, 512], F32)) for i in range(2)]
        VO = [es.enter_context(P.sbuf("vo%d" % i, [128, 512], BF16)) for i in range(2)]
        GO = [es.enter_context(P.sbuf("go%d" % i, [128, 32], F32)) for i in range(2)]
        GT = [es.enter_context(P.sbuf("gt%d" % i, [128, 16], F32)) for i in range(2)]
        for ti in range(NTILE):
            t0 = ti * 128
            bi = min(t0 // 512, 8)
            pz, pv, pa = PZ[ti % 2], PV[ti % 2], PA[ti % 2]
            zo, vo, go, gt = ZO[ti % 2], VO[ti % 2], GO[ti % 2], GT[ti % 2]
            for kc in range(8):
                P.mm(pz[:, :], hT.k(bi)[:, kc, t0:t0 + 128], WZ[:, kc, :], start=(kc == 0), stop=(kc == 7))
            for kc in range(8):
                P.mm(pv[:, :], hT.k(bi)[:, kc, t0:t0 + 128], WV[:, kc, :], start=(kc == 0), stop=(kc == 7))
            for kc in range(8):
                P.mm(pa[:, :], hT.k(bi)[:, kc, t0:t0 + 128], WA[:, kc, :], start=(kc == 0), stop=(kc == 7))
            P.act(zo[:, :], pz[:, :], AF.Silu)
            P.dma("sp", Opnd(("ZS", ti), K.ZS.h[t0:t0 + 128, :]), zo[:, :])
            P.cp(vo[:, :], pv[:, :])
            P.dma("sp", Opnd(("NV", ti), K.NV.h[t0:t0 + 128, :]), vo[:, :])
            P.tt(gt[:, :], pa[:, 0:16], sm["DTB"][:, :], ALU.add)
            P.act(gt[:, :], gt[:, :], AF.Exp)
            P.act(gt[:, :], gt[:, :], AF.Ln, bias=1.0)
            P.tt(go[:, 0:16], gt[:, :], NEGA[:, :], ALU.mult)
            P.act(go[:, 16:32], pa[:, 16:32], AF.Sigmoid)
            P.dma("sp", Opnd(("GB", ti), K.GB.h[t0:t0 + 128, :]), go[:, :])
        P.barrier()
    with ExitStack() as es:
        WS = [es.enter_context(P.sbuf("ws%d" % i, [128, 8, 128], F32)) for i in range(2)]
        WB = [es.enter_context(P.sbuf("wb%d" % i, [128, 8, 128], BF16)) for i in range(2)]
        PS = [es.enter_context(P.psum("ips%d" % i, [128, 512], F32)) for i in range(2)]
        PS2 = [es.enter_context(P.psum("ips2%d" % i, [128, 512], F32)) for i in range(2)]
        RAW = es.enter_context(P.sbuf("raw", [128, RAWLEN], F32))
        Y = es.enter_context(P.sbuf("convy", [128, T], F32))
        COS = es.enter_context(P.sbuf("cos", [128, LT], F32))
        SIN = es.enter_context(P.sbuf("sin", [128, LT], F32))
        T1 = [es.enter_context(P.sbuf("rt1%d" % i, [128, 512], F32)) for i in range(2)]
        T2 = [es.enter_context(P.sbuf("rt2%d" % i, [128, 512], F32)) for i in range(2)]
        T3 = [es.enter_context(P.sbuf("rt3%d" % i, [128, 512], F32)) for i in range(2)]
        OB = [es.enter_context(P.sbuf("rob%d" % i, [128, 512], BF16)) for i in range(2)]
        P.dma("sp", COS[:, :], K.dCOS[:, :])
        P.dma("sp", SIN[:, :], K.dSIN[:, :])
        P.memset(RAW[:, :], 0.0)
        cnt = [0]

        def chunk_mm(col0, bi):
            pass

        nchunk = 0
        for ci in range(12 + 8 + 16):
            if ci < 12:
                col0 = C_DQKV + ci * 128
            elif ci < 20:
                col0 = C_NQ + (ci - 12) * 128
            else:
                col0 = C_GA + (ci - 20) * 128
            ws, wb = WS[ci % 2], WB[ci % 2]
            load_w_bf16(P, K, ("w_in", l), win[:, :, col0:col0 + 128], ws, wb, 128)
            for bi, (t0, n) in enumerate(BLOCKS):
                cnt[0] += 1
                u = cnt[0] % 2
                ps = PS[u]
                for kc in range(8):
                    P.mm(ps[:, :n], wb[:, kc, :], hT.k(bi)[:, kc, t0:t0 + n], start=(kc == 0), stop=(kc == 7))
                if ci < 12:
                    r0 = RAW_L0 + t0 if bi < 8 else RAW_C0
                    P.cp(RAW.k(bi)[:, r0:r0 + n], ps[:, :n], e="act") if False else P.act(RAW.k(bi)[:, r0:r0 + n], ps[:, :n], AF.Copy)
                elif ci < 20:
                    isq = ci < 16
                    t1, t2, t3, ob, ps2 = T1[u], T2[u], T3[u], OB[u], PS2[u]
                    P.act(t1[:, :n], ps[:, :n], AF.Copy)
                    P.act(t2[:, :n], ps[:, :n], AF.Square)
                    P.mm(ps2[:, :n], c["BLK"][:, :], t2[:, :n])
                    P.rsq(t3[:, :n], ps2[:, :n], 1.0 / 64.0, EPS)
                    P.stt(ob[:, :n], t1[:, :n], sm["NAW"][:, (0 if isq else 1):(1 if isq else 2)], t3[:, :n], ALU.mult, ALU.mult)
                    if isq:
                        P.ts(ob[:, :n], ob[:, :n], 0.125, ALU.mult)
                    dst = K.NQT if isq else K.NKT
                    r = ((ci - 12) % 4) * 128
                    P.dma("sp", Opnd((dst.name, ci, bi), dst.h[r:r + 128, t0:t0 + n]), ob[:, :n])
                else:
                    t1 = T1[u]
                    P.act(t1[:, :n], ps[:, :n], AF.Sigmoid)
                    r = (ci - 20) * 128
                    P.dma("sp", Opnd(("SG", ci, bi), K.SG.h[r:r + 128, t0:t0 + n]), t1[:, :n])
            if ci < 12:
                cw = sm["CONVW"]
                for (y0, r0, n) in ((0, RAW_L0 - 2, LT), (LT, RAW_C0 - 2, CT)):
                    rr = [RAW.k(b) for b in range(9)]
                    ins_all = [RAW.k(b)[:, 0:1] for b in range(9)]
                    for j in range(5):
                        src = Opnd(("RAWALL",), RAW.h[:, r0 + j:r0 + j + n])
                        if j == 0:
                            P.op("dve", lambda g, s=src, j=j: g.tensor_scalar(Y.h[:, y0:y0 + n], s.ap, cw.h[:, ci, j:j + 1], None, ALU.mult),
                                 [Y.k(y0)[:, y0:y0 + n]], ins_all + [cw[:, ci, :]])
                        else:
                            P.op("dve", lambda g, s=src, j=j: g.scalar_tensor_tensor(Y.h[:, y0:y0 + n], s.ap, cw.h[:, ci, j:j + 1], Y.h[:, y0:y0 + n], ALU.mult, ALU.add),
                                 [Y.k(y0)[:, y0:y0 + n]], ins_all + [cw[:, ci, :], Y.k(y0)[:, y0:y0 + n]])
                    P.act(Y.k(y0)[:, y0:y0 + n], Y.k(y0)[:, y0:y0 + n], AF.Silu)
                if ci < 8:
                    for bi, (t0, n) in enumerate(BLOCKS):
                        cnt[0] += 1
                        u = cnt[0] % 2
                        yk = Y.k(0 if bi < 8 else LT)
                        t1, t2, t3, ps2, ps = T1[u], T2[u], T3[u], PS2[u], PS[u]
                        P.act(t1[:, :n], yk[:, t0:t0 + n], AF.Square)
                        P.mm(ps2[:, :n], c["BLK"][:, :], t1[:, :n])
                        P.rsq(t2[:, :n], ps2[:, :n], 1.0, EPS)
                        if bi < 8:
                            P.tt(t3[:, :n], yk[:, t0:t0 + n], t2[:, :n], ALU.mult)
                            P.mm(ps[:, :n], c["ROT"][:, :], t3[:, :n])
                            P.tt(t1[:, :n], ps[:, :n], SIN[:, t0:t0 + n], ALU.mult)
                            P.tt(t3[:, :n], t3[:, :n], COS[:, t0:t0 + n], ALU.mult)
                            P.tt(t3[:, :n], t3[:, :n], t1[:, :n], ALU.add)
                        else:
                            P.tt(t3[:, :n], yk[:, t0:t0 + n], t2[:, :n], ALU.mult)
                        P.dma("sp", Opnd(("DQKVT", ci, bi), K.DQKVT.h[ci * 128:(ci + 1) * 128, t0:t0 + n]), t3[:, :n])
                else:
                    P.dma("sp", Opnd(("DQKVT", ci, 0), K.DQKVT.h[ci * 128:(ci + 1) * 128, 0:LT]), Y.k(0)[:, 0:LT])
                    P.dma("sp", Opnd(("DQKVT", ci, 8), K.DQKVT.h[ci * 128:(ci + 1) * 128, LT:T]), Y.k(LT)[:, LT:T])
        P.barrier()


SCRATCH = {"XT": ((D, T), F32), "ZS": ((T, 512), F32), "NV": ((T, 512), BF16), "GB": ((T, 32), F32),
           "DQKVT": ((1536, T), F32), "NQT": ((512, T), BF16), "NKT": ((512, T), BF16), "SG": ((2048, T), F32),
           "ODN": ((2, T, 512), F32), "OBT": ((512, T), BF16), "FT": ((T, D), F32)}


def build(nlayers=DEPTH, stop_after=None, debug=()):
    nc = bass.Bass("TRN2", target_bir_lowering=False)
    K = Ctx()
    dram_in(nc, K, "XT0", (D, T))
    dram_in(nc, K, "CIN", (128, 8, 2))
    for n, shp in BIG_SHAPES.items():
        dram_in(nc, K, n, shp)
    dconst = {n: dram_in(nc, K, "c_" + n, shp) for n, shp in CONST_SHAPES.items()}
    dsmall = [{n: dram_in(nc, K, "s%d_%s" % (l, n), shp) for n, shp in SMALL_SHAPES.items()} for l in range(DEPTH)]
    K.dBIAST = [dram_in(nc, K, "s%d_BIAST" % l, BIAST_SHAPE) for l in range(DEPTH)]
    K.dCOS, K.dSIN = dconst["COS"], dconst["SIN"]
    for n, (shp, dt) in SCRATCH.items():
        dram_scratch(nc, K, n, shp, dt, kind=("ExternalOutput" if n in debug else "Internal"))
    dram_scratch(nc, K, "YT", (D, LT), F32, kind="ExternalOutput")
    with ExitStack() as es:
        P = Prog(nc, es)
        K.c = {}
        for n, shp in CONST_SHAPES.items():
            if n in ("COS", "SIN"):
                continue
            K.c[n] = es.enter_context(P.sbuf("k" + n, shp, F32))
            P.dma("sp", K.c[n][tuple(slice(None) for _ in shp)], dconst[n][tuple(slice(None) for _ in shp)])
        K.small = {n: es.enter_context(P.sbuf("sm" + n, shp, F32)) for n, shp in SMALL_SHAPES.items()}
        K.SC = es.enter_context(P.sbuf("SC", [128, 8, 2], F32))
        K.MOD = es.enter_context(P.sbuf("MOD", [128, 48, 2], F32))
        K.A1 = es.enter_context(P.sbuf("A1", [128, 8, 2], F32))
        K.A2 = es.enter_context(P.sbuf("A2", [128, 8, 2], F32))
        P.dma("sp", K.SC[:, :, :], K.CIN[:, :, :])
        P.act(K.SC[:, :, :], K.SC[:, :, :], AF.Silu)
        for bi, (t0, n) in enumerate(BLOCKS):
            P.dma("sp", Opnd(("XT", bi), K.XT.h[:, t0:t0 + n]), Opnd("XT0", K.XT0.h[:, t0:t0 + n]))

        def stop(tag):
            return stop_after == tag

        for l in range(nlayers):
            for n, shp in SMALL_SHAPES.items():
                sl = tuple(slice(None) for _ in shp)
                P.dma("sp", K.small[n][sl], dsmall[l][n][sl])
            stage_mod(P, K, l)
            if stop("mod"):
                break
            with P.sbuf("hT", [128, 8, T], BF16) as hT:
                stage_norm(P, K, K.A1, 0, hT)
                stage_inproj(P, K, l, hT)
                P.barrier()
            if stop("inproj"):
                break
            if "skipdn" not in debug:
                stage_dn(P, K)
            if stop("dn"):
                break
            stage_na(P, K, l)
            if stop("na"):
                break
            stage_merge(P, K, l)
            if stop("merge"):
                break
            stage_peer(P, K, l, tiles=getattr(K, "peer_tiles", None))
            if stop("peer"):
                break
        P.barrier()
        if "MODOUT" in debug:
            dram_scratch(nc, K, "MODOUT", (128, 48, 2), F32, kind="ExternalOutput")
            P.dma("sp", K.MODOUT[:, :, :], K.MOD[:, :, :])
        for bi in range(8):
            t0 = bi * 512
            P.dma("sp", Opnd(("YT", bi), K.YT.h[:, t0:t0 + 512]), Opnd(("XT", bi), K.XT.h[:, t0:t0 + 512]))
        P.barrier()
        print("instructions:", P.ninst, flush=True)
    return nc


def make_in_maps(inputs):
    inp = {k: np.asarray(v) for k, v in inputs.items()}
    consts = _consts()
    shared = {}
    for n in BIG_SHAPES:
        if n.startswith("peer_u") or n.startswith("peer_v"):
            shared[n] = np.ascontiguousarray(inp[n[:6]][int(n[6:])], dtype=np.float32)
        else:
            shared[n] = np.ascontiguousarray(inp[n], dtype=np.float32)
    for n, v in consts.items():
        shared["c_" + n] = np.ascontiguousarray(v, dtype=np.float32)
    for l in range(DEPTH):
        for n, v in _layer_small(inp, l).items():
            shared["s%d_%s" % (l, n)] = np.ascontiguousarray(v, dtype=np.float32)
    maps = []
    for b in range(8):
        m = dict(shared)
        xt = np.concatenate([inp["x"][b].T, inp["ctx"][b].T], axis=1)
        m["XT0"] = np.ascontiguousarray(xt, dtype=np.float32)
        cin = np.stack([inp["c"][b].reshape(8, 128).T, inp["c_ctx"].reshape(8, 128).T], axis=2)
        m["CIN"] = np.ascontiguousarray(cin, dtype=np.float32)
        maps.append(m)
    return maps


def kernel(**inputs):
    nc = build()
    maps = make_in_maps(inputs)
    res = run_bass_kernel_spmd(nc, maps, core_ids=list(range(8)))
    out = np.stack([np.ascontiguousarray(res.results[b]["YT"].T) for b in range(8)], axis=0)
    return out.astype(np.float32)


def bc(o, axis, shape):
    return Opnd(o.key, o.ap.unsqueeze(axis).to_broadcast(list(shape)))


def stage_dn(P, K):
    c = K.c
    steps = [(LT, 4, i) for i in range(4)] + [(0, 64, i) for i in range(64)]
    DQ = K.DQKVT.h
    GBv = K.GB.h.rearrange("t (a d h) -> t a d h", a=2, d=2)
    SH = [128, 8, 128]
    with ExitStack() as es:
        def sb(n, s=SH, dt=F32):
            return es.enter_context(P.sbuf(n, s, dt))
        KBD = [sb("kbd%d" % i) for i in range(2)]
        QBD = [sb("qbd%d" % i) for i in range(2)]
        VT2 = [sb("vt2%d" % i, [64, 8, 128]) for i in range(2)]
        GBt = [sb("gbt%d" % i, [128, 2, 8]) for i in range(2)]
        X = [es.enter_context(P.psum("dx%d" % i, [128, 1024], F32)) for i in range(4)]
        S = sb("S", [128, 8, 64])
        GC = sb("GC", [128, 16])
        EG = sb("EG", [128, 8])
        EGL = sb("EGL", [128, 8])
        EDGL = sb("EDGL", [128, 8])
        GL, DT, DECI, DECS, EGB, XM, XMT, INTRAT, QD, KG, KDEC, R, UT = [sb(n) for n in (
            "GL", "DT", "DECI", "DECS", "EGB", "XM", "XMT", "INTRAT", "QD", "KG", "KDEC", "R", "UT")]
        Yb = [sb("Y%d" % i) for i in range(2)]
        YTb = [sb("YT%d" % i) for i in range(2)]
        VS, BW, T1, VN, OO = [sb(n, [128, 8, 64]) for n in ("VS", "BW", "T1", "VN", "OO")]
        for t_ in KBD + QBD:
            P.memset(t_[:, :, :], 0.0, e="pool")
        P.memset(S[:, :, :], 0.0)
        for si, (base, nch, i) in enumerate(steps):
            u = si % 2
            tf = base + 64 * i
            tb = base + 64 * (nch - 1 - i)
            kbd, qbd, vt2, gbt = KBD[u], QBD[u], VT2[u], GBt[u]
            for (half, t0) in ((0, tf), (1, tb)):
                ps_ = slice(64 * half, 64 * half + 64)
                P.dma("sp", kbd[ps_, :, ps_], Opnd("DQKVT", DQ[512:1024, t0:t0 + 64].rearrange("(h d) t -> d h t", h=8)))
                P.dma("sp", qbd[ps_, :, ps_], Opnd("DQKVT", DQ[0:512, t0:t0 + 64].rearrange("(h d) t -> d h t", h=8)))
                P.dma("sp", vt2[:, :, ps_], Opnd("DQKVT", DQ[1024:1536, t0:t0 + 64].rearrange("(h d) t -> d h t", h=8)))
                P.dma("sp", gbt[ps_, :, :], Opnd("GB", GBv[t0:t0 + 64, :, half, :]))
            gs = gbt[:, 0, :]
            be = gbt[:, 1, :]
            P.mm(X[0][:, 0:8], c["TRI"][:, :], gs)
            P.mm(X[0][:, 8:16], c["BLK"][:, :], gs)
            P.cp(GC[:, :], X[0][:, 0:16])
            P.act(EG[:, :], GC[:, 0:8], AF.Exp)
            P.act(EGL[:, :], GC[:, 8:16], AF.Exp)
            P.tt(EDGL[:, :], GC[:, 8:16], GC[:, 0:8], ALU.subtract)
            P.act(EDGL[:, :], EDGL[:, :], AF.Exp)
            P.tt(GL[:, :, :], bc(c["BLK"][:, :], 1, SH), bc(gs, 2, SH), ALU.mult)
            x1 = X[1][:, :].ap.rearrange("p (h c) -> p h c", h=8)
            X1 = Opnd(X[1].name, x1)
            for h in range(8):
                P.mm(Opnd(X[1].name, x1[:, h, :]), GL[:, h, :], c["TRI"][:, :])
            P.tt(DT[:, :, :], X1, bc(GC[:, 0:8], 2, SH), ALU.subtract)
            P.ts(DT[:, :, :], DT[:, :, :], 0.0, ALU.min)
            P.act(DT[:, :, :], DT[:, :, :], AF.Exp)
            P.tt(DECI[:, :, :], DT[:, :, :], bc(c["TRI"][:, :], 1, SH), ALU.mult, e="pool")
            P.tt(DECS[:, :, :], DT[:, :, :], bc(c["STRI"][:, :], 1, SH), ALU.mult)
            P.act(EGB[:, :, :], X1, AF.Exp)
            x2 = X[2][:, :].ap.rearrange("p (h c) -> p h c", h=8)
            x3 = X[3][:, :].ap.rearrange("p (h c) -> p h c", h=8)
            x0 = X[0][:, :].ap.rearrange("p (h c) -> p h c", h=8)
            X0, X2, X3 = Opnd(X[0].name, x0), Opnd(X[2].name, x2), Opnd(X[3].name, x3)
            for h in range(8):
                P.mm(Opnd(X[2].name, x2[:, h, :]), kbd[:, h, :], kbd[:, h, :])
            for h in range(8):
                P.mm(Opnd(X[3].name, x3[:, h, :]), kbd[:, h, :], qbd[:, h, :])
            P.tt(XM[:, :, :], X2, DECS[:, :, :], ALU.mult)
            P.tt(XM[:, :, :], XM[:, :, :], bc(be, 2, SH), ALU.mult)
            P.stt(INTRAT[:, :, :], X3, 0.125, DECI[:, :, :], ALU.mult, ALU.mult)
            P.stt(QD[:, :, :], qbd[:, :, :], 0.125, EGB[:, :, :], ALU.mult, ALU.mult)
            for h in range(8):
                P.tr(Opnd(X[0].name, x0[:, h, :]), kbd[:, h, :], c["IDF"][:, :])
            P.tt(KG[:, :, :], X0, bc(EG[:, :], 2, SH), ALU.mult)
            P.tt(KDEC[:, :, :], X0, bc(EDGL[:, :], 2, SH), ALU.mult)
            x1v = X[1][:, 0:512].ap.rearrange("p (h c) -> p h c", h=8)
            for h in range(8):
                P.tr(Opnd(X[1].name, x1v[:, h, :]), vt2[:, h, :], c["IDF"][0:64, 0:64])
            P.act(VS[:, :, :], Opnd(X[1].name, x1v), AF.Copy)
            for h in range(8):
                P.tr(Opnd(X[2].name, x2[:, h, :]), XM[:, h, :], c["IDF"][:, :])
            P.act(XMT[:, :, :], X2, AF.Copy)
            P.tt(R[:, :, :], bc(c["IDF"][:, :], 1, SH), XM[:, :, :], ALU.subtract)
            yp, ytp = XM, XMT
            for lv in range(5):
                yn, ytn = Yb[lv % 2], YTb[lv % 2]
                last = lv == 4
                if not last:
                    for h in range(8):
                        P.mm(Opnd(X[3].name, x3[:, h, :]), ytp[:, h, :], yp[:, h, :])
                    P.act(yn[:, :, :], X3, AF.Copy)
                for h in range(8):
                    P.mm(Opnd(X[0].name, x0[:, h, :]), yp[:, h, :], ytp[:, h, :])
                P.cp(ytn[:, :, :], X0)
                for h in range(8):
                    P.mm(Opnd(X[2].name, x2[:, h, :]), ytn[:, h, :], R[:, h, :])
                P.tt(R[:, :, :], R[:, :, :], X2, ALU.add)
                yp, ytp = yn, ytn
            for h in range(8):
                P.mm(Opnd(X[3].name, x3[:, h, :]), KG[:, h, :], R[:, h, :])
            P.act(UT[:, :, :], X3, AF.Copy)
            for h in range(8):
                P.mm(Opnd(X[1].name, x1v[:, h, :]), R[:, h, :], VS[:, h, :])
            S64 = [128, 8, 64]
            P.tt(BW[:, :, :], Opnd(X[1].name, x1v), bc(be, 2, S64), ALU.mult)
            x0v = X[0][:, 0:512].ap.rearrange("p (h c) -> p h c", h=8)
            x2v = X[2][:, 0:512].ap.rearrange("p (h c) -> p h c", h=8)
            x3v = X[3][:, 0:512].ap.rearrange("p (h c) -> p h c", h=8)
            for h in range(8):
                P.mm(Opnd(X[0].name, x0v[:, h, :]), UT[:, h, :], S[:, h, :])
            P.tt(T1[:, :, :], Opnd(X[0].name, x0v), bc(be, 2, S64), ALU.mult)
            P.tt(VN[:, :, :], BW[:, :, :], T1[:, :, :], ALU.subtract)
            for h in range(8):
                P.mm(Opnd(X[2].name, x2v[:, h, :]), QD[:, h, :], S[:, h, :], start=True, stop=False)
                P.mm(Opnd(X[2].name, x2v[:, h, :]), INTRAT[:, h, :], VN[:, h, :], start=False, stop=True)
            P.act(OO[:, :, :], Opnd(X[2].name, x2v), AF.Copy)
            for h in range(8):
                P.mm(Opnd(X[3].name, x3v[:, h, :]), KDEC[:, h, :], VN[:, h, :])
            P.tt(S[:, :, :], S[:, :, :], bc(EGL[:, :], 2, S64), ALU.mult)
            P.tt(S[:, :, :], S[:, :, :], Opnd(X[3].name, x3v), ALU.add)
            oov = OO.h[:, :, :].rearrange("p h e -> p (h e)")
            P.dma("sp", Opnd(("ODN", 0, tf), K.ODN.h[0, tf:tf + 64, :]), Opnd(OO.name, oov[0:64, :]))
            P.dma("sp", Opnd(("ODN", 1, tb), K.ODN.h[1, tb:tb + 64, :]), Opnd(OO.name, oov[64:128, :]))
        P.barrier()


def stage_na(P, K, l):
    c = K.c
    with ExitStack() as es:
        def sb(n, s, dt=F32):
            return es.enter_context(P.sbuf(n, s, dt))
        NKT = sb("nkt", [128, 4, T], BF16)
        NQT = sb("nqt", [128, 4, T], BF16)
        NV = sb("nv", [128, NTILE, 512], BF16)
        OBT = sb("obt", [128, 4, T], BF16)
        BIASB = sb("biasb", [128, 8, 16, 64], BF16)
        IDB = sb("idb", [128, 128], BF16)
        ONESB = sb("onesb", [128, 128], BF16)
        PT = [sb("pt%d" % i, [128, 7, 64], BF16) for i in range(2)]
        RD = [sb("rd%d" % i, [128, 64], F32) for i in range(2)]
        PS = [es.enter_context(P.psum("nps%d" % i, [128, 7, 64], F32)) for i in range(2)]
        PO = [es.enter_context(P.psum("npo%d" % i, [128, 64], F32)) for i in range(2)]
        PD = [es.enter_context(P.psum("npd%d" % i, [128, 64], F32)) for i in range(2)]
        for cch in range(4):
            P.dma("sp", NKT.k(cch)[:, cch, :], Opnd("NKT", K.NKT.h[cch * 128:(cch + 1) * 128, :]))
            P.dma("sp", NQT.k(cch)[:, cch, :], Opnd("NQT", K.NQT.h[cch * 128:(cch + 1) * 128, :]))
        nvv = K.NV.h.rearrange("(n p) f -> p n f", p=128)
        for g4 in range(0, NTILE, 2):
            P.dma("sp", NV.k(g4)[:, g4:g4 + 2, :], Opnd("NV", nvv[:, g4:g4 + 2, :]))
        with P.sbuf("biasf", [128, 4, 16, 64], F32) as BIASF:
            for hh in range(2):
                P.dma("sp", BIASF[:, :, :, :], Opnd(("BIAST", l), K.dBIAST[l].h[:, hh * 4:(hh + 1) * 4, :, :]))
                P.cp(BIASB[:, hh * 4:(hh + 1) * 4, :, :], BIASF[:, :, :, :], e="pool")
            P.barrier()
        P.cp(IDB[:, :], c["IDF"][:, :])
        P.memset(ONESB[:, :], 1.0)
        if hasattr(K, "na_rows"):
            P.memset(OBT[:, :, :], 0.0, e="pool")
            P.barrier()
        it = 0
        for r in getattr(K, 'na_rows', range(68)):
            if r < 64:
                loc = na_sched(r)
                chunks = [(kt, tbl) for kt, tbl in loc] + [(32, None), (33, None)]
                q0 = r * 64
            else:
                chunks = [(32, None), (33, None)]
                q0 = LT + (r - 64) * 64
            nch = len(chunks)
            for h in range(8):
                hc, hp = h // 2, h % 2
                pr = slice(64 * hp, 64 * hp + 64)
                u = it % 2
                it += 1
                ps, po, pd, pt, rd = PS[u], PO[u], PD[u], PT[u], RD[u]
                for j, (kt, tbl) in enumerate(chunks):
                    has_b = tbl is not None
                    P.mm(ps.k(j)[:, j, :], NKT.k(hc)[pr, hc, kt * 128:(kt + 1) * 128], NQT.k(hc)[pr, hc, q0:q0 + 64],
                         start=True, stop=not has_b)
                    if has_b:
                        P.mm(ps.k(j)[:, j, :], IDB[:, :], BIASB[:, h, tbl, :], start=False, stop=True)
                psall = Opnd(ps.name, ps.h[:, 0:nch, :])
                P.op("act", lambda g, a=pt.h[:, 0:nch, :], b=ps.h[:, 0:nch, :]: g.activation(a, b, AF.Exp),
                     [pt[:, :, :]], [ps.k(j)[:, j, :] for j in range(nch)])
                for j, (kt, tbl) in enumerate(chunks):
                    g4 = kt - (kt % 2)
                    P.mm(po[:, :], NV.k(g4)[:, kt, hc * 128:(hc + 1) * 128], pt[:, j, :], start=(j == 0), stop=(j == nch - 1))
                for j, (kt, tbl) in enumerate(chunks):
                    P.mm(pd[:, :], ONESB[:, :], pt[:, j, :], start=(j == 0), stop=(j == nch - 1))
                P.op("dve", lambda g, a=rd.h[pr, :], b=pd.h[pr, :]: g.reciprocal(a, b), [rd[:, :]], [pd[:, :]])
                P.op("dve", lambda g, a=OBT.h[pr, hc, q0:q0 + 64], b=po.h[pr, :], c_=rd.h[pr, :]: g.tensor_tensor(a, b, c_, ALU.mult),
                     [OBT.k((r, h))[:, 0, 0:1]], [po[:, :], rd[:, :]])
        P.barrier()
        for cch in range(4):
            P.dma("sp", Opnd("OBTd", K.OBT.h[cch * 128:(cch + 1) * 128, :]), Opnd(OBT.name, OBT.h[:, cch, :]))
        P.barrier()


def stage_merge(P, K, l):
    c = K.c
    sm = K.small
    XTv = K.XT.h.rearrange("(fc p) t -> p fc t", p=128)
    SGv = K.SG.h.rearrange("(g n p) t -> p g n t", g=2, p=128)
    OBv = K.OBT.h.rearrange("(kc p) t -> p kc t", p=128)
    with ExitStack() as es:
        def sb(n, s, dt=F32):
            return es.enter_context(P.sbuf(n, s, dt))
        WS = sb("mws", [128, 4, 1024], F32)
        WPA = sb("wpa", [128, 4, 1024], BF16)
        WPB = sb("wpb", [128, 4, 1024], BF16)
        WO = sb("wo", [128, 8, 1024], BF16)
        IDB = sb("idb2", [128, 128], BF16)
        OAT = sb("oat", [128, 4, T], BF16)
        P.cp(IDB[:, :], c["IDF"][:, :])
        wpa = K.w_pa.h[l].rearrange("(kc p) n -> p kc n", p=128)
        wpb = K.w_pb.h[l].rearrange("(kc p) n -> p kc n", p=128)
        wo = K.w_out.h[l].rearrange("(kc p) n -> p kc n", p=128)
        P.dma("sp", WS[:, :, :], Opnd(("w_pa", l), wpa))
        P.cp(WPA[:, :, :], WS[:, :, :], e="pool")
        P.dma("sp", WS[:, :, :], Opnd(("w_pb", l), wpb))
        P.cp(WPB[:, :, :], WS[:, :, :], e="pool")
        for hh in range(2):
            P.dma("sp", WS[:, :, :], Opnd(("w_out", l), wo[:, hh * 4:(hh + 1) * 4, :]))
            P.cp(WO[:, hh * 4:(hh + 1) * 4, :], WS[:, :, :], e="pool")
        with ExitStack() as es2:
            def sb2(n, s, dt=F32):
                return es2.enter_context(P.sbuf(n, s, dt))
            O0 = [sb2("o0%d" % i, [128, 512]) for i in range(2)]
            O1 = [sb2("o1%d" % i, [128, 512]) for i in range(2)]
            ZT = [sb2("zt%d" % i, [128, 512]) for i in range(2)]
            SQ = [sb2("osq%d" % i, [128, 512]) for i in range(2)]
            SS = [sb2("oss%d" % i, [128, 8]) for i in range(2)]
            OA = [sb2("oa%d" % i, [128, 512], BF16) for i in range(2)]
            PT_ = [es2.enter_context(P.psum("mpt%d" % i, [128, 512], BF16)) for i in range(2)]
            S3 = [128, 8, 64]
            for ti in range(NTILE):
                u = ti % 2
                t0 = ti * 128
                o0, o1, zt, sq, ss, oa, pt = O0[u], O1[u], ZT[u], SQ[u], SS[u], OA[u], PT_[u]
                P.dma("sp", o0[:, :], Opnd("ODN", K.ODN.h[0, t0:t0 + 128, :]))
                P.dma("sp", o1[:, :], Opnd("ODN", K.ODN.h[1, t0:t0 + 128, :]))
                P.dma("sp", zt[:, :], Opnd("ZS", K.ZS.h[t0:t0 + 128, :]))
                P.tt(o0[:, :], o0[:, :], o1[:, :], ALU.add)
                P.act(sq[:, :], o0[:, :], AF.Square)
                P.red(ss[:, :], Opnd(sq.name, sq.h[:, :].rearrange("p (h e) -> p h e", h=8)), ALU.add)
                P.rsq(ss[:, :], ss[:, :], 1.0 / 64.0, EPS)
                o3 = Opnd(o0.name, o0.h[:, :].rearrange("p (h e) -> p h e", h=8))
                P.tt(o3, o3, bc(ss[:, :], 2, S3), ALU.mult)
                P.tt(o3, o3, bc(sm["DNW"][:, :], 1, S3), ALU.mult)
                P.tt(oa[:, :], o0[:, :], zt[:, :], ALU.mult)
                for kc in range(4):
                    P.tr(pt.k(kc)[:, kc * 128:(kc + 1) * 128], oa[:, kc * 128:(kc + 1) * 128], IDB[:, :])
                P.op("act", lambda g, a=OAT.h[:, :, t0:t0 + 128], b=pt.h[:, :].rearrange("p (k t) -> p k t", k=4): g.activation(a, b, AF.Copy),
                     [OAT.k(ti)[:, 0, 0:1]], [pt.k(kc)[:, 0:1] for kc in range(4)])
            P.barrier()
        with ExitStack() as es2:
            def sb2(n, s, dt=F32):
                return es2.enter_context(P.sbuf(n, s, dt))
            OB = [sb2("mob%d" % i, [128, 4, 512], BF16) for i in range(2)]
            XB = [sb2("mxb%d" % i, [128, 8, 512]) for i in range(2)]
            SGA = [sb2("sga%d" % i, [128, 512]) for i in range(2)]
            SGB = [sb2("sgb%d" % i, [128, 512]) for i in range(2)]
            TA = [sb2("mta%d" % i, [128, 512]) for i in range(2)]
            TB = [sb2("mtb%d" % i, [128, 512]) for i in range(2)]
            YT = [sb2("myt%d" % i, [128, 8, 512], BF16) for i in range(2)]
            PA = [es2.enter_context(P.psum("mpa%d" % i, [128, 512], F32)) for i in range(2)]
            PB = [es2.enter_context(P.psum("mpb%d" % i, [128, 512], F32)) for i in range(2)]
            PO = [es2.enter_context(P.psum("mpo%d" % i, [128, 512], F32)) for i in range(2)]
            it = 0
            for bi, (t0, n) in enumerate(BLOCKS):
                j = 1 if bi == 8 else 0
                ob, xb, yt = OB[bi % 2], XB[bi % 2], YT[bi % 2]
                P.dma("sp", ob[:, :, :n], Opnd("OBTd", OBv[:, :, t0:t0 + n]))
                P.dma("sp", xb[:, :, :n], Opnd(("XT", bi), XTv[:, :, t0:t0 + n]))
                for nn in range(8):
                    u = it % 2
                    it += 1
                    sga, sgb, ta, tb, pa, pb = SGA[u], SGB[u], TA[u], TB[u], PA[u], PB[u]
                    P.dma("sp", sga[:, :n], Opnd("SG", SGv[:, 0, nn, t0:t0 + n]))
                    P.dma("sp", sgb[:, :n], Opnd("SG", SGv[:, 1, nn, t0:t0 + n]))
                    for kc in range(4):
                        P.mm(pa[:, :n], WPA[:, kc, nn * 128:(nn + 1) * 128], Opnd(OAT.name, OAT.h[:, kc, t0:t0 + n]), start=(kc == 0), stop=(kc == 3))
                    for kc in range(4):
                        P.mm(pb[:, :n], WPB[:, kc, nn * 128:(nn + 1) * 128], ob[:, kc, :n], start=(kc == 0), stop=(kc == 3))
                    P.tt(ta[:, :n], pa[:, :n], sga[:, :n], ALU.mult)
                    P.tt(tb[:, :n], pb[:, :n], sgb[:, :n], ALU.mult)
                    P.tt(yt.k(nn)[:, nn, :n], ta[:, :n], tb[:, :n], ALU.add, e="pool")
                for m in range(8):
                    po = PO[m % 2]
                    for nn in range(8):
                        P.mm(po[:, :n], WO[:, nn, m * 128:(m + 1) * 128], yt.k(nn)[:, nn, :n], start=(nn == 0), stop=(nn == 7))
                    g1 = modv(K, 2, m, j)
                    P.op("dve", lambda g, a=xb.h[:, m, :n], b=po.h[:, :n], s_=g1.ap: g.scalar_tensor_tensor(a, b, s_, a, ALU.mult, ALU.add),
                         [xb.k(m)[:, m, 0:1]], [po[:, :n], g1, xb[:, 0, 0:1]])
                P.dma("sp", Opnd(("XT", bi), XTv[:, :, t0:t0 + n]), Opnd(xb.name, xb.h[:, :, :n]),
                      extra_reads=[xb.k(m)[:, m, 0:1] for m in range(8)])
            P.barrier()


def stage_peer(P, K, l, tiles=None):
    c = K.c
    sm = K.small
    XTv = K.XT.h.rearrange("(fc p) t -> p fc t", p=128)
    wq = K.peer_wq.h[l].rearrange("(kc p) n -> p kc n", p=128)
    Utab = Opnd(("peer_u", l), getattr(K, "peer_u%d" % l).h[:, :])
    Vtab = Opnd(("peer_v", l), getattr(K, "peer_v%d" % l).h[:, :])
    NB = 16
    with ExitStack() as es:
        def sb(n, s, dt=F32):
            return es.enter_context(P.sbuf(n, s, dt))
        WQ = sb("wq", [128, 8, 2048], BF16)
        with P.sbuf("pws", [128, 8, 512], F32) as WS:
            for q4 in range(4):
                P.dma("sp", WS[:, :, :], Opnd(("peer_wq", l), wq[:, :, q4 * 512:(q4 + 1) * 512]))
                P.cp(WQ[:, :, q4 * 512:(q4 + 1) * 512], WS[:, :, :], e="pool")
            P.barrier()
        IDB = sb("idb3", [128, 128], BF16)
        P.cp(IDB[:, :], c["IDF"][:, :])
        XB = [sb("pxb%d" % i, [128, 8, 128]) for i in range(2)]
        SQ = sb("psq", [128, 8, 128])
        RS = sb("prs", [128, 128])
        TMP = [sb("ptmp%d" % i, [128, 128]) for i in range(2)]
        HT = sb("pht", [128, 8, 128], BF16)
        QTs = sb("qts", [128, 16, 128])
        SCR = sb("scr", [128, 16, 128])
        SCR2 = sb("scr2", [128, 16, 128])
        TS = sb("tsv", [128, 16, 16])
        TIu = sb("tiu", [128, 16, 16], U32)
        TIf = sb("tif", [128, 16, 16])
        CS = sb("cs", [128, 8, 256])
        CS2 = sb("cs2", [128, 8, 256])
        BS = sb("bs", [128, 8, 16])
        POSu = sb("posu", [128, 8, 16], U32)
        Au = sb("au", [128, 8, 16], U32)
        Bu = sb("bu", [128, 8, 16], U32)
        Af = sb("af", [128, 8, 16])
        Bf = sb("bf", [128, 8, 16])
        EQ = sb("eq", [128, 8, 16, 16])
        ISel = sb("isel", [128, 8, 16])
        JSel = sb("jsel", [128, 8, 16])
        IDXf = sb("idxf", [128, 128])
        IDX = sb("idx", [128, 128], I32)
        DD = sb("dd", [128, 8, 16])
        SMs = sb("sms", [128, 8])
        GATE = sb("gate", [128, 8, 16])
        HTOK = sb("htok", [128, 1024])
        UG = [sb("ug%d" % i, [128, 1024]) for i in range(NB)]
        JUNK = [sb("junk%d" % i, [128, 1024], BF16) for i in range(4)]
        APRE = sb("apre", [128, 128])
        G1 = sb("g1", [128, 128])
        G2 = sb("g2", [128, 128])
        COEF = sb("coef", [128, 128])
        DIAG = [sb("diag%d" % i, [128, 128]) for i in range(4)]
        FTOK = sb("ftok", [128, 1024])
        ACC = sb("acc", [128, 1024])
        PQ = es.enter_context(P.psum("ppq", [128, 128], F32))
        PACC = es.enter_context(P.psum("pacc", [128, 1024], F32))
        PSC = es.enter_context(P.psum("psc", [128, 8, 128], F32))
        PH = es.enter_context(P.psum("pph", [128, 1024], BF16))
        PF = es.enter_context(P.psum("ppf", [128, 8, 128], F32))
        S4 = [128, 8, 16, 16]
        for ti in (tiles if tiles is not None else range(NTILE)):
            t0 = ti * 128
            j = 1 if t0 >= LT else 0
            xb = XB[ti % 2]
            P.dma("sp", xb[:, :, :], Opnd(("XT", min(t0 // 512, 8)), XTv[:, :, t0:t0 + 128]))
            P.act(SQ[:, :, :], xb[:, :, :], AF.Square)
            for fc in range(8):
                P.mm(PSC[:, 0, :], c["ONESM"][:, :], SQ[:, fc, :], start=(fc == 0), stop=(fc == 7))
            P.rsq(RS[:, :], PSC[:, 0, :], 1.0, EPS)
            for fc in range(8):
                tmp = TMP[fc % 2]
                P.stt(tmp[:, :], xb[:, fc, :], K.A2[:, fc, j:j + 1], RS[:, :], ALU.mult, ALU.mult)
                P.act(HT.k(fc)[:, fc, :], tmp[:, :], AF.Identity, bias=modv(K, 3, fc, j))
            for hp in range(16):
                for kc in range(8):
                    P.mm(PQ[:, :], WQ[:, kc, hp * 128:(hp + 1) * 128], HT.k(kc)[:, kc, :], start=(kc == 0), stop=(kc == 7))
                if hp % 2 == 0:
                    P.act(QTs.k(hp)[:, hp, :], PQ[:, :], AF.Copy)
                else:
                    P.cp(QTs.k(hp)[:, hp, :], PQ[:, :])
            for half in range(2):
                for h8 in range(8):
                    hp = half * 8 + h8
                    P.mm(PSC[:, h8, :], QTs.k(hp)[:, hp, :], sm["KEYST"][:, hp, :])
                P.act(SCR.k(half)[:, half * 8:(half + 1) * 8, :], PSC[:, :, :], AF.Copy)
            for hp in range(16):
                P.op("dve", lambda g, hp=hp: g.max(TS.h[:, hp, 0:8], SCR.h[:, hp, :]), [TS.k(hp)[:, hp, 0:8]], [SCR.k(hp // 8)[:, hp, :]])
                P.op("dve", lambda g, hp=hp: g.max_index(TIu.h[:, hp, 0:8], TS.h[:, hp, 0:8], SCR.h[:, hp, :]), [TIu.k(hp)[:, hp, 0:8]], [TS.k(hp)[:, hp, 0:8], SCR.k(hp // 8)[:, hp, :]])
                P.op("dve", lambda g, hp=hp: g.match_replace(SCR2.h[:, hp, :], TS.h[:, hp, 0:8], SCR.h[:, hp, :], -1e30), [SCR2.k(hp)[:, hp, :]], [TS.k(hp)[:, hp, 0:8], SCR.k(hp // 8)[:, hp, :]])
                P.op("dve", lambda g, hp=hp: g.max(TS.h[:, hp, 8:16], SCR2.h[:, hp, :]), [TS.k((hp, 1))[:, hp, 8:16]], [SCR2.k(hp)[:, hp, :]])
                P.op("dve", lambda g, hp=hp: g.max_index(TIu.h[:, hp, 8:16], TS.h[:, hp, 8:16], SCR2.h[:, hp, :]), [TIu.k((hp, 1))[:, hp, 8:16]], [TS.k((hp, 1))[:, hp, 8:16], SCR2.k(hp)[:, hp, :]])
            ts_all = [TS.k(hp)[:, hp, 0:8] for hp in range(16)] + [TS.k((hp, 1))[:, hp, 8:16] for hp in range(16)]
            ti_all = [TIu.k(hp)[:, hp, 0:8] for hp in range(16)] + [TIu.k((hp, 1))[:, hp, 8:16] for hp in range(16)]
            P.op("dve", lambda g: g.tensor_copy(TIf.h[:, :, :], TIu.h[:, :, :]), [TIf[:, :, :]], ti_all)
            ts4 = TS.h[:, :, :].rearrange("p (h two) k -> p h two k", two=2)
            ti4 = TIf.h[:, :, :].rearrange("p (h two) k -> p h two k", two=2)
            cs4 = CS.h[:, :, :].rearrange("p h (a b) -> p h a b", a=16)
            P.op("dve", lambda g: g.tensor_tensor(cs4, ts4[:, :, 0, :].unsqueeze(3).to_broadcast(S4), ts4[:, :, 1, :].unsqueeze(2).to_broadcast(S4), ALU.add),
                 [CS[:, :, :]], ts_all)
            for h in range(8):
                P.op("dve", lambda g, h=h: g.max(BS.h[:, h, 0:8], CS.h[:, h, :]), [BS.k(h)[:, h, 0:8]], [CS[:, h, :]])
                P.op("dve", lambda g, h=h: g.max_index(POSu.h[:, h, 0:8], BS.h[:, h, 0:8], CS.h[:, h, :]), [POSu.k(h)[:, h, 0:8]], [BS.k(h)[:, h, 0:8], CS[:, h, :]])
                P.op("dve", lambda g, h=h: g.match_replace(CS2.h[:, h, :], BS.h[:, h, 0:8], CS.h[:, h, :], -1e30), [CS2.k(h)[:, h, :]], [BS.k(h)[:, h, 0:8], CS[:, h, :]])
                P.op("dve", lambda g, h=h: g.max(BS.h[:, h, 8:16], CS2.h[:, h, :]), [BS.k((h, 1))[:, h, 8:16]], [CS2.k(h)[:, h, :]])
                P.op("dve", lambda g, h=h: g.max_index(POSu.h[:, h, 8:16], BS.h[:, h, 8:16], CS2.h[:, h, :]), [POSu.k((h, 1))[:, h, 8:16]], [BS.k((h, 1))[:, h, 8:16], CS2.k(h)[:, h, :]])
            bs_all = [BS.k(h)[:, h, 0:8] for h in range(8)] + [BS.k((h, 1))[:, h, 8:16] for h in range(8)]
            pos_all = [POSu.k(h)[:, h, 0:8] for h in range(8)] + [POSu.k((h, 1))[:, h, 8:16] for h in range(8)]
            P.op("dve", lambda g: g.tensor_scalar(Au.h[:, :, :], POSu.h[:, :, :], 4, None, ALU.logical_shift_right), [Au[:, :, :]], pos_all)
            P.op("dve", lambda g: g.tensor_scalar(Bu.h[:, :, :], POSu.h[:, :, :], 15, None, ALU.bitwise_and), [Bu[:, :, :]], pos_all)
            P.cp(Af[:, :, :], Au[:, :, :])
            P.cp(Bf[:, :, :], Bu[:, :, :])
            io4 = c["IOTA16"].h[:, :].unsqueeze(1).unsqueeze(1).to_broadcast(S4)
            for (sel, xf, which) in ((ISel, Af, 0), (JSel, Bf, 1)):
                P.op("dve", lambda g, xf=xf: g.tensor_tensor(EQ.h[:, :, :, :], io4, xf.h[:, :, :].unsqueeze(3).to_broadcast(S4), ALU.is_equal),
                     [EQ[:, :, :, :]], [xf[:, :, :], c["IOTA16"][:, :]])
                P.op("dve", lambda g, which=which: g.tensor_tensor(EQ.h[:, :, :, :], EQ.h[:, :, :, :], ti4[:, :, which, :].unsqueeze(2).to_broadcast(S4), ALU.mult),
                     [EQ[:, :, :, :]], [EQ[:, :, :, :], TIf[:, :, :]])
                P.red(sel[:, :, :], EQ[:, :, :, :], ALU.add)
            idx3 = IDXf.h[:, :].rearrange("p (h k) -> p h k", h=8)
            P.op("dve", lambda g: g.scalar_tensor_tensor(idx3, ISel.h[:, :, :], 128.0, JSel.h[:, :, :], ALU.mult, ALU.add),
                 [IDXf[:, :]], [ISel[:, :, :], JSel[:, :, :]])
            P.cp(IDX[:, :], IDXf[:, :])
            P.op("dve", lambda g: g.tensor_tensor(DD.h[:, :, :], BS.h[:, :, :], BS.h[:, :, 0:1].to_broadcast([128, 8, 16]), ALU.subtract),
                 [DD[:, :, :]], bs_all)
            P.act(DD[:, :, :], DD[:, :, :], AF.Exp)
            P.red(SMs[:, :], DD[:, :, :], ALU.add)
            P.op("dve", lambda g: g.reciprocal(SMs.h[:, :], SMs.h[:, :]), [SMs[:, :]], [SMs[:, :]])
            P.tt(GATE[:, :, :], DD[:, :, :], bc(SMs[:, :], 2, [128, 8, 16]), ALU.mult)
            for fc in range(8):
                P.tr(PH.k(fc)[:, fc * 128:(fc + 1) * 128], HT.k(fc)[:, fc, :], IDB[:, :])
            P.op("act", lambda g: g.activation(HTOK.h[:, :], PH.h[:, :], AF.Copy), [HTOK[:, :]], [PH.k(fc)[:, 0:1] for fc in range(8)])
            for k in range(128):
                buf = UG[k % NB]
                P.gather(buf[:, :], Utab, IDX[:, k:k + 1], 16384)
                jk = JUNK[k % 4]
                P.op("dve", lambda g, k=k, buf=buf, jk=jk: g.scalar_tensor_tensor(jk.h[:, :], HTOK.h[:, :], 1.0, buf.h[:, :], ALU.mult, ALU.mult, accum_out=APRE.h[:, k:k + 1]),
                     [jk[:, :], APRE.k(k)[:, k:k + 1]], [HTOK[:, :], buf[:, :]])
            apre_all = [APRE.k(k)[:, k:k + 1] for k in range(128)]
            P.op("dve", lambda g: g.tensor_tensor(G1.h[:, :], APRE.h[:, :], APRE.h[:, :], ALU.mult), [G1[:, :]], apre_all)
            P.op("dve", lambda g: g.tensor_tensor(G1.h[:, :], G1.h[:, :], APRE.h[:, :], ALU.mult), [G1[:, :]], apre_all + [G1[:, :]])
            P.op("dve", lambda g: g.scalar_tensor_tensor(G2.h[:, :], G1.h[:, :], 0.044715, APRE.h[:, :], ALU.mult, ALU.add), [G2[:, :]], apre_all + [G1[:, :]])
            P.act(G2[:, :], G2[:, :], AF.Sigmoid, scale=1.5957691216057308)
            P.op("dve", lambda g: g.tensor_tensor(G1.h[:, :], G2.h[:, :], APRE.h[:, :], ALU.mult), [G1[:, :]], apre_all + [G2[:, :]])
            P.tt(COEF[:, :], G1[:, :], Opnd(GATE.name, GATE.h[:, :, :].rearrange("p h k -> p (h k)")), ALU.mult)
            for k in range(128):
                buf = UG[k % NB]
                P.gather(buf[:, :], Vtab, IDX[:, k:k + 1], 16384)
                if k % 2 == 0:
                    dg = DIAG[(k // 2) % 4]
                    P.ts(dg[:, :], c["IDF"][:, :], COEF[:, k:k + 1], ALU.mult)
                    P.mm(PACC[:, 0:512], dg[:, :], buf[:, 0:512], start=(k == 0), stop=(k == 126))
                    P.mm(PACC[:, 512:1024], dg[:, :], buf[:, 512:1024], start=(k == 0), stop=(k == 126))
                elif k == 1:
                    P.ts(ACC[:, :], buf[:, :], COEF[:, k:k + 1], ALU.mult)
                else:
                    P.stt(ACC[:, :], buf[:, :], COEF[:, k:k + 1], ACC[:, :], ALU.mult, ALU.add)
            P.tt(FTOK[:, :], PACC[:, :], ACC[:, :], ALU.add)
            for fc in range(8):
                P.tr(PF.k(fc)[:, fc, :], FTOK[:, fc * 128:(fc + 1) * 128], c["IDF"][:, :])
            for fc in range(8):
                g2 = modv(K, 5, fc, j)
                P.op("dve", lambda g, fc=fc, g2=g2, xb=xb: g.scalar_tensor_tensor(xb.h[:, fc, :], PF.h[:, fc, :], g2.ap, xb.h[:, fc, :], ALU.mult, ALU.add),
                     [xb.k(fc)[:, fc, 0:1]], [PF.k(f_)[:, f_, :] for f_ in range(8)] + [g2, xb[:, 0, 0:1]])
            P.dma("sp", Opnd(("XT", min(t0 // 512, 8)), XTv[:, :, t0:t0 + 128]), Opnd(xb.name, xb.h[:, :, :]),
                  extra_reads=[xb.k(fc)[:, fc, 0:1] for fc in range(8)])
        P.barrier()
```
